# Optimizing a Trainium2 kernel written in Bass

```python
import jax
import jax.numpy as jnp
from jax import lax


D_MODEL = 1024
BATCH = 2
SEQ = 8192
DEPTH = 2

GRID_W = 64
CTX_LEN = 256
NORM_EPS = 1e-6
GLA_HEADS = 4
GLA_DK = 64
GLA_DV = 128
GLA_KW = GLA_HEADS * GLA_DK
GLA_WIDTH = GLA_HEADS * GLA_DV
GLA_GATE_RANK = 16
GLA_TAU = 16.0
GLA_CHUNK = 64
SWA_HEADS = 8
SWA_KV_HEADS = 2
SWA_GROUP = SWA_HEADS // SWA_KV_HEADS
SWA_HD = 64
SWA_WIDTH = SWA_HEADS * SWA_HD
SWA_KVW = SWA_KV_HEADS * SWA_HD
WINDOW = 128
ATT_BLOCK = 128
ROPE_THETA = 10000.0
ROPE_AXIS_DIM = SWA_HD // 2
MIX_WIDTH = GLA_WIDTH + SWA_WIDTH
N_EXPERTS = 16
N_GROUPS = 4
EXPERTS_PER_GROUP = N_EXPERTS // N_GROUPS
TOP_K = 2
D_EXPERT = 512
MOE_BLOCK = 256
IN_SIZES = (GLA_KW, GLA_KW, GLA_WIDTH, GLA_WIDTH, GLA_GATE_RANK, GLA_GATE_RANK, SWA_WIDTH, SWA_KVW, SWA_KVW)
IN_COLS = sum(IN_SIZES)

kernel_name = 'hybrid_gla_swa_moe_dit_block'


def rmsnorm(x, g):
    xf = x.astype(jnp.float32)
    y = xf * lax.rsqrt(jnp.mean(xf * xf, axis=-1, keepdims=True) + NORM_EPS)
    return (y * g.astype(jnp.float32)).astype(x.dtype)


def split_in_proj(p):
    points, acc = [], 0
    for s in IN_SIZES[:-1]:
        acc += s
        points.append(acc)
    return jnp.split(p, points, axis=-1)


def axial_rope_tables(n_tok):
    rows = n_tok // GRID_W
    row = jnp.repeat(jnp.arange(rows, dtype=jnp.float32), GRID_W)
    col = jnp.tile(jnp.arange(GRID_W, dtype=jnp.float32), rows)
    n_freq = ROPE_AXIS_DIM // 2
    inv = ROPE_THETA ** (-(jnp.arange(n_freq, dtype=jnp.float32) * 2.0 / ROPE_AXIS_DIM))
    ang_r = row[:, None] * inv[None, :]
    ang_c = col[:, None] * inv[None, :]
    return (jnp.cos(ang_r), jnp.sin(ang_r), jnp.cos(ang_c), jnp.sin(ang_c))


def rotate_half(x, cos, sin):
    x1, x2 = jnp.split(x, 2, axis=-1)
    return jnp.concatenate([x1 * cos - x2 * sin, x2 * cos + x1 * sin], axis=-1)


def apply_axial_rope(x, tabs):
    shape = (1, x.shape[1]) + (1,) * (x.ndim - 3) + (ROPE_AXIS_DIM // 2,)
    cr, sr, cc, sc = [t.reshape(shape).astype(x.dtype) for t in tabs]
    x_row, x_col = jnp.split(x, 2, axis=-1)
    return jnp.concatenate([rotate_half(x_row, cr, sr), rotate_half(x_col, cc, sc)], axis=-1)


def gla_chunked(q, k, v, log_a, s0):
    B, H, L, dk = q.shape
    dv = v.shape[-1]
    C = GLA_CHUNK
    N = L // C
    q = q.reshape(B, H, N, C, dk)
    k = k.reshape(B, H, N, C, dk)
    v = v.reshape(B, H, N, C, dv)
    b = jnp.cumsum(log_a.reshape(B, H, N, C, dk), axis=3)
    b_last = b[:, :, :, -1:, :]
    q_in = q * jnp.exp(b)
    k_in = k * jnp.exp(-b)
    k_out = k * jnp.exp(b_last - b)
    causal_in_chunk = jnp.tril(jnp.ones((C, C), dtype=bool))
    A = jnp.where(causal_in_chunk, jnp.einsum('bhncd,bhnsd->bhncs', q_in, k_in), 0.0)
    o_intra = jnp.einsum('bhncs,bhnse->bhnce', A, v)
    U = jnp.einsum('bhncd,bhnce->bhnde', k_out, v)
    dec = jnp.exp(b_last[:, :, :, 0, :])

    def step(S, inp):
        u, d = inp
        return d[..., None] * S + u, S

    s_final, s_prev = lax.scan(step, s0, (jnp.moveaxis(U, 2, 0), jnp.moveaxis(dec, 2, 0)))
    s_prev = jnp.moveaxis(s_prev, 0, 2)
    o_inter = jnp.einsum('bhncd,bhnde->bhnce', q_in, s_prev)
    return (o_intra + o_inter).reshape(B, H, L, dv), s_final


def gla_heads(q, k, v, z_f, z_b, up_f, bias_f, up_b, bias_b):
    B, L, _ = q.shape

    def heads(t, d):
        return t.reshape(B, L, GLA_HEADS, d).transpose(0, 2, 1, 3).astype(jnp.float32)

    log_a_f = jax.nn.log_sigmoid((z_f @ up_f + bias_f).astype(jnp.float32)) / GLA_TAU
    log_a_b = jax.nn.log_sigmoid((z_b @ up_b + bias_b).astype(jnp.float32)) / GLA_TAU
    return (heads(q, GLA_DK) * GLA_DK ** -0.5, heads(k, GLA_DK), heads(v, GLA_DV),
            heads(log_a_f, GLA_DK), heads(log_a_b, GLA_DK))


def gla_bidir(q, k, v, la_f, la_b, s_f0, s_b0):
    o_f, s_f = gla_chunked(q, k, v, la_f, s_f0)
    fl = lambda t: jnp.flip(t, axis=2)
    o_b, s_b = gla_chunked(fl(q), fl(k), fl(v), fl(la_b), s_b0)
    return o_f + fl(o_b), s_f, s_b


def gla_output(o, gate, norm_g):
    B, H, L, dv = o.shape
    o = o * lax.rsqrt(jnp.mean(o * o, axis=-1, keepdims=True) + NORM_EPS)
    o = o.transpose(0, 2, 1, 3).reshape(B, L, H * dv) * norm_g.astype(jnp.float32)
    return o.astype(gate.dtype) * jax.nn.silu(gate)


def swa_latent(q, k, v, kc, vc, sink):
    B, L, KV, G, hd = q.shape
    W = ATT_BLOCK
    N = L // W
    scale = hd ** -0.5
    qb = q.reshape(B, N, W, KV, G, hd)
    pad = ((0, 0), (W, W), (0, 0), (0, 0))
    kp = jnp.pad(k, pad).reshape(B, N + 2, W, KV, hd)
    vp = jnp.pad(v, pad).reshape(B, N + 2, W, KV, hd)
    kb = jnp.concatenate([kp[:, :-2], kp[:, 1:-1], kp[:, 2:]], axis=2)
    vb = jnp.concatenate([vp[:, :-2], vp[:, 1:-1], vp[:, 2:]], axis=2)
    qpos = jnp.arange(N)[:, None, None] * W + jnp.arange(W)[None, :, None]
    kpos = jnp.arange(N)[:, None, None] * W - W + jnp.arange(3 * W)[None, None, :]
    valid = (jnp.abs(kpos - qpos) <= WINDOW) & (kpos >= 0) & (kpos < L)
    s_loc = jnp.einsum('bnqhgd,bnkhd->bhgnqk', qb, kb).astype(jnp.float32) * scale
    s_loc = jnp.where(valid, s_loc, -jnp.inf)
    s_ctx = jnp.einsum('bnqhgd,bkhd->bhgnqk', qb, kc).astype(jnp.float32) * scale
    sk = sink.reshape(KV, G)[None, :, :, None, None, None].astype(jnp.float32)
    m = jnp.maximum(jnp.maximum(s_loc.max(-1, keepdims=True), s_ctx.max(-1, keepdims=True)), sk)
    p_loc = jnp.exp(s_loc - m)
    p_ctx = jnp.exp(s_ctx - m)
    denom = p_loc.sum(-1, keepdims=True) + p_ctx.sum(-1, keepdims=True) + jnp.exp(sk - m)
    o = (jnp.einsum('bhgnqk,bnkhd->bnqhgd', p_loc, vb.astype(jnp.float32))
         + jnp.einsum('bhgnqk,bkhd->bnqhgd', p_ctx, vc.astype(jnp.float32)))
    o = o / denom.transpose(0, 3, 4, 1, 2, 5)
    return o.reshape(B, L, KV * G * hd).astype(v.dtype)


def swa_context(q, k, v, sink):
    B, Lc, KV, G, hd = q.shape
    s = jnp.einsum('bqhgd,bkhd->bhgqk', q, k).astype(jnp.float32) * hd ** -0.5
    sk = sink.reshape(KV, G)[None, :, :, None, None].astype(jnp.float32)
    m = jnp.maximum(s.max(-1, keepdims=True), sk)
    p = jnp.exp(s - m)
    p = p / (p.sum(-1, keepdims=True) + jnp.exp(sk - m))
    o = jnp.einsum('bhgqk,bkhd->bqhgd', p, v.astype(jnp.float32))
    return o.reshape(B, Lc, KV * G * hd).astype(v.dtype)


def moe(h, w_router, b_router, w_gate, w_up, w_down):
    T, D = h.shape
    s = jax.nn.sigmoid((h @ w_router).astype(jnp.float32))
    sb = s + b_router.astype(jnp.float32)
    g_score = lax.top_k(sb.reshape(T, N_GROUPS, EXPERTS_PER_GROUP), 2)[0].sum(-1)
    g_sel = jnp.argmax(g_score, axis=-1)
    in_group = (jnp.arange(N_EXPERTS) // EXPERTS_PER_GROUP)[None, :] == g_sel[:, None]
    _, idx = lax.top_k(jnp.where(in_group, sb, -jnp.inf), TOP_K)
    w_sel = jnp.take_along_axis(s, idx, axis=-1)
    w_sel = w_sel / w_sel.sum(-1, keepdims=True)
    M = MOE_BLOCK
    A = T * TOP_K
    e_flat = idx.reshape(A)
    tok_flat = jnp.repeat(jnp.arange(T), TOP_K)
    w_flat = w_sel.reshape(A)
    order = jnp.argsort(e_flat)
    e_s, tok_s, w_s = e_flat[order], tok_flat[order], w_flat[order]
    counts = jnp.bincount(e_flat, length=N_EXPERTS)
    padded = ((counts + M - 1) // M) * M
    pend = jnp.cumsum(padded)
    pstart = pend - padded
    ustart = jnp.cumsum(counts) - counts
    dest = pstart[e_s] + jnp.arange(A) - ustart[e_s]
    n_blocks = -(-A // M) + N_EXPERTS
    P = n_blocks * M
    row_tok = jnp.full((P,), T, dtype=jnp.int32).at[dest].set(tok_s.astype(jnp.int32))
    row_w = jnp.zeros((P,), jnp.float32).at[dest].set(w_s)
    block_e = jnp.minimum(jnp.searchsorted(pend, jnp.arange(n_blocks) * M, side='right'), N_EXPERTS - 1)
    h_pad = jnp.concatenate([h, jnp.zeros((1, D), h.dtype)], axis=0)
    xb = h_pad[row_tok].reshape(n_blocks, M, D)

    def expert_block(args):
        xblk, e = args
        return (jax.nn.silu(xblk @ w_gate[e]) * (xblk @ w_up[e])) @ w_down[e]

    yb = lax.map(expert_block, (xb, block_e)).reshape(P, D)
    y = jax.ops.segment_sum(yb.astype(jnp.float32) * row_w[:, None], row_tok, num_segments=T + 1)[:T]
    return y.astype(h.dtype)


def setup_inputs(seed: int = 0) -> dict:
    key = jax.random.key(seed)
    ks = jax.random.split(key, 24)
    f32 = jnp.float32
    D = D_MODEL

    def nrm(k, shape, s):
        return jax.random.normal(k, shape, f32) * s

    return {
        'x': nrm(ks[0], (BATCH, SEQ, D), 1.0),
        'c': nrm(ks[1], (BATCH, D), 1.0),
        'ctx': nrm(ks[2], (BATCH, CTX_LEN, D), 1.0),
        'c_ctx': nrm(ks[3], (D,), 1.0),
        'w_ada': nrm(ks[4], (DEPTH, D, 6 * D), 0.5 * D ** -0.5),
        'b_ada': nrm(ks[5], (DEPTH, 6 * D), 0.02),
        'norm1': 1.0 + nrm(ks[6], (DEPTH, D), 0.02),
        'norm2': 1.0 + nrm(ks[7], (DEPTH, D), 0.02),
        'w_in': nrm(ks[8], (DEPTH, D, IN_COLS), D ** -0.5),
        'gla_up_f': nrm(ks[9], (DEPTH, GLA_GATE_RANK, GLA_KW), GLA_GATE_RANK ** -0.5),
        'gla_bias_f': nrm(ks[10], (DEPTH, GLA_KW), 0.1),
        'gla_up_b': nrm(ks[11], (DEPTH, GLA_GATE_RANK, GLA_KW), GLA_GATE_RANK ** -0.5),
        'gla_bias_b': nrm(ks[12], (DEPTH, GLA_KW), 0.1),
        'gla_norm': 1.0 + nrm(ks[13], (DEPTH, GLA_WIDTH), 0.02),
        'swa_sink': nrm(ks[14], (DEPTH, SWA_HEADS), 1.0),
        'w_out': nrm(ks[15], (DEPTH, MIX_WIDTH, D), MIX_WIDTH ** -0.5),
        'w_router': nrm(ks[16], (D, N_EXPERTS), D ** -0.5),
        'b_router': nrm(ks[17], (N_EXPERTS,), 0.01),
        'w_gate': nrm(ks[18], (DEPTH, N_EXPERTS, D, D_EXPERT), D ** -0.5),
        'w_up': nrm(ks[19], (DEPTH, N_EXPERTS, D, D_EXPERT), D ** -0.5),
        'w_down': nrm(ks[20], (DEPTH, N_EXPERTS, D_EXPERT, D), D_EXPERT ** -0.5),
        'final_norm': 1.0 + nrm(ks[21], (D,), 0.02),
    }


def reference(x, c, ctx, c_ctx, w_ada, b_ada, norm1, norm2, w_in, gla_up_f, gla_bias_f, gla_up_b,
              gla_bias_b, gla_norm, swa_sink, w_out, w_router, b_router, w_gate, w_up, w_down, final_norm):
    B, L, D = x.shape
    Lc = ctx.shape[1]
    rope_tabs = axial_rope_tables(L)
    c_act = jax.nn.silu(c)
    cc_act = jax.nn.silu(c_ctx)
    xl, xc = x, ctx
    for i in range(DEPTH):
        last = i == DEPTH - 1
        mod_l = (c_act @ w_ada[i] + b_ada[i])[:, None, :]
        mod_c = (cc_act @ w_ada[i] + b_ada[i])[None, None, :]
        sh1, sc1, g1, sh2, sc2, g2 = jnp.split(mod_l, 6, axis=-1)
        csh1, csc1, cg1, csh2, csc2, cg2 = jnp.split(mod_c, 6, axis=-1)

        hl = rmsnorm(xl, norm1[i]) * (1.0 + sc1) + sh1
        hc = rmsnorm(xc, norm1[i]) * (1.0 + csc1) + csh1
        lq, lk, lv, lg, lzf, lzb, lsq, lsk, lsv = split_in_proj(hl @ w_in[i])
        cq, ck, cv, cg, czf, czb, csq, csk, csv = split_in_proj(hc @ w_in[i])

        gq, gk, gv, gaf, gab = gla_heads(cq, ck, cv, czf, czb, gla_up_f[i], gla_bias_f[i], gla_up_b[i], gla_bias_b[i])
        s_zero = jnp.zeros((B, GLA_HEADS, GLA_DK, GLA_DV), jnp.float32)
        o_gla_c, s_f, s_b = gla_bidir(gq, gk, gv, gaf, gab, s_zero, s_zero)
        gq, gk, gv, gaf, gab = gla_heads(lq, lk, lv, lzf, lzb, gla_up_f[i], gla_bias_f[i], gla_up_b[i], gla_bias_b[i])
        o_gla_l, _, _ = gla_bidir(gq, gk, gv, gaf, gab, s_f, s_b)
        gla_l = gla_output(o_gla_l, lg, gla_norm[i])

        q_l = apply_axial_rope(lsq.reshape(B, L, SWA_KV_HEADS, SWA_GROUP, SWA_HD), rope_tabs)
        k_l = apply_axial_rope(lsk.reshape(B, L, SWA_KV_HEADS, SWA_HD), rope_tabs)
        v_l = lsv.reshape(B, L, SWA_KV_HEADS, SWA_HD)
        k_c = csk.reshape(B, Lc, SWA_KV_HEADS, SWA_HD)
        v_c = csv.reshape(B, Lc, SWA_KV_HEADS, SWA_HD)
        swa_l = swa_latent(q_l, k_l, v_l, k_c, v_c, swa_sink[i])

        xl = xl + g1 * (jnp.concatenate([gla_l, swa_l], axis=-1) @ w_out[i])

        h2l = rmsnorm(xl, norm2[i]) * (1.0 + sc2) + sh2
        if last:
            xl = xl + g2 * moe(h2l.reshape(B * L, D), w_router, b_router, w_gate[i], w_up[i], w_down[i]).reshape(B, L, D)
        else:
            gla_c = gla_output(o_gla_c, cg, gla_norm[i])
            q_c = csq.reshape(B, Lc, SWA_KV_HEADS, SWA_GROUP, SWA_HD)
            swa_c = swa_context(q_c, k_c, v_c, swa_sink[i])
            xc = xc + cg1 * (jnp.concatenate([gla_c, swa_c], axis=-1) @ w_out[i])
            h2c = rmsnorm(xc, norm2[i]) * (1.0 + csc2) + csh2
            tokens = jnp.concatenate([h2l.reshape(B * L, D), h2c.reshape(B * Lc, D)], axis=0)
            y = moe(tokens, w_router, b_router, w_gate[i], w_up[i], w_down[i])
            xl = xl + g2 * y[:B * L].reshape(B, L, D)
            xc = xc + cg2 * y[B * L:].reshape(B, Lc, D)
    return rmsnorm(xl, final_norm)
```

```python
import contextlib
import numpy as np
import ml_dtypes
import concourse.bass as bass
import concourse.mybir as mybir
from concourse.bass_utils import run_bass_kernel_spmd

F32 = mybir.dt.float32
BF16 = mybir.dt.bfloat16
ALU = mybir.AluOpType
AF = mybir.ActivationFunctionType
AX = mybir.AxisListType
NPBF = ml_dtypes.bfloat16

D = 1024
NT_L = 16
NT_C = 2
NT = NT_L + NT_C
TL = NT_L * 128
EPS = 1e-6
NEXP = 16
DE = 512
ARENA_WORDS = 39600


class Tok:
    __slots__ = ("w", "r", "name", "excl")

    def __init__(self, name="", excl=False):
        self.w = None
        self.r = []
        self.name = name
        self.excl = excl


class Op:
    __slots__ = ("eng", "fn", "deps", "sig", "chan", "has_dep", "seq", "inc")

    def __init__(self, eng, fn, deps, chan, inc=16):
        self.eng = eng
        self.fn = fn
        self.deps = deps
        self.sig = None
        self.chan = chan
        self.has_dep = False
        self.seq = 0
        self.inc = inc


class Prog:
    ENGS = ("pe", "act", "dve", "pool", "sp")

    def __init__(self, nc):
        self.nc = nc
        self.ops = {e: [] for e in self.ENGS}
        self.stack = contextlib.ExitStack()
        self.nt = 0
        self.banks = []
        self.bank_i = 0
        self.nchan = 0
        self.nseq = 0
        self.cont = False
        self.arena = None
        self.in_scope = False
        self.arena_off = 0
        self.arena_words = 0

    def sb(self, name, shape, dt):
        if self.in_scope:
            nel = 1
            for d_ in shape[1:]:
                nel *= d_
            esz = 2 if dt == BF16 else 4
            words = (nel * esz + 3) // 4
            off = self.arena_off
            assert off + words <= self.arena_words, (name, off, words, self.arena_words)
            self.arena_off += words
            v = self.arena[0:shape[0], off:off + words]
            if dt == BF16:
                v = v.bitcast(BF16)[:, 0:nel]
            elif dt != F32:
                v = v.bitcast(dt)
            if len(shape) > 2:
                names = " ".join(f"a{i}" for i in range(len(shape) - 1))
                kw = {f"a{i}": shape[i + 1] for i in range(len(shape) - 2)}
                v = v.rearrange(f"p ({names}) -> p {names}", **kw)
            return v
        return self.stack.enter_context(self.nc.sbuf_tensor("sb_" + name, list(shape), dt))

    def open_scope(self, words=None):
        if self.arena is None:
            self.arena_words = words
            self.arena = self.stack.enter_context(self.nc.sbuf_tensor("sb_arena", [128, words], F32))
        self.arena_off = 0
        self.in_scope = True

    def close_scope(self):
        self.barrier()
        self.in_scope = False

    def ps(self, name, shape, dt=F32):
        return self.stack.enter_context(self.nc.psum_tensor("ps_" + name, list(shape), dt))

    def tok(self, name="", excl=False):
        self.nt += 1
        return Tok(name or f"t{self.nt}", excl)

    def newchan(self, name="c"):
        self.nchan += 1
        return f"{name}{self.nchan}"

    def make_banks(self, n):
        for i in range(n):
            self.banks.append((self.ps(f"bank{i}", [128, 512], F32), self.tok(f"bank{i}", True)))

    def bank(self):
        b = self.banks[self.bank_i % len(self.banks)]
        self.bank_i += 1
        return b

    def op(self, eng, fn, reads=(), writes=(), chan=None, inc=16):
        deps = []
        ex = [t for t in reads if t.excl]
        if ex:
            reads = [t for t in reads if not t.excl]
            writes = list(writes) + ex
        for t in reads:
            if t.w is not None:
                deps.append(t.w)
        for t in writes:
            if t.w is not None:
                deps.append(t.w)
            deps.extend(t.r)
        if eng == "pe" and self.cont:
            deps = [d for d in deps if d.eng != "pe"]
        self.cont = False
        o = Op(eng, fn, deps, chan, inc)
        self.nseq += 1
        o.seq = self.nseq
        for d in deps:
            d.has_dep = True
        for t in reads:
            t.r.append(o)
        for t in writes:
            t.w = o
            t.r = []
        self.ops[eng].append(o)
        return o

    def barrier(self):
        last = []
        for e in self.ENGS:
            for o in reversed(self.ops[e]):
                if o.chan is None and o.fn is not None:
                    last.append(o)
                    break
        seen = set()
        for e in self.ENGS:
            for o in reversed(self.ops[e]):
                if o.chan is not None and o.chan not in seen:
                    seen.add(o.chan)
                    last.append(o)
        for e in self.ENGS:
            o = Op(e, None, list(last), None)
            self.nseq += 1
            o.seq = self.nseq
            self.ops[e].append(o)
        for d in last:
            d.has_dep = True

    def dma(self, eng, out, in_, reads=(), writes=(), chan=None, **kw):
        assert chan is not None
        return self.op(eng, lambda e: e.dma_start(out=out, in_=in_, **kw), reads, writes, chan=chan)

    def emit(self, final_wait_eng="sp"):
        nc = self.nc
        sems = {}
        for e in self.ENGS:
            sems[e] = self.stack.enter_context(nc.semaphore(f"s_{e}"))
        cnt = {e: 0 for e in self.ENGS}
        ccnt = {}
        for e in ("pe", "act", "dve", "pool"):
            for o in reversed(self.ops[e]):
                if o.chan is None and o.fn is not None:
                    o.has_dep = True
                    break
        allops = sorted((o for e in self.ENGS for o in self.ops[e]), key=lambda o: o.seq)
        for o in allops:
            e = o.eng
            if o.chan is not None:
                if o.chan not in sems:
                    sems[o.chan] = self.stack.enter_context(nc.semaphore(f"c_{o.chan}"))
                    ccnt[o.chan] = 0
                ccnt[o.chan] += o.inc
                o.sig = (o.chan, ccnt[o.chan], o.inc)
            elif o.has_dep and o.fn is not None:
                cnt[e] += 1
                o.sig = (e, cnt[e], 1)
        final = dict(ccnt)
        for e in ("pe", "act", "dve", "pool"):
            if cnt[e]:
                final[e] = cnt[e]

        def run(e, handle, extra_final):
            waited = {}
            for o in self.ops[e]:
                need = {}
                for d in o.deps:
                    k, v, _ = d.sig
                    if need.get(k, 0) < v:
                        need[k] = v
                for k, v in need.items():
                    if waited.get(k, 0) < v:
                        handle.wait_ge(sems[k], v)
                        waited[k] = v
                if o.fn is None:
                    continue
                ins = o.fn(handle)
                if o.sig is not None:
                    ins.then_inc(sems[o.sig[0]], o.sig[2])
            if extra_final:
                for k, v in final.items():
                    if waited.get(k, 0) < v:
                        handle.wait_ge(sems[k], v)

        with nc.Block() as block:
            @block.tensor
            def _(h):
                run("pe", h, False)

            @block.scalar
            def _(h):
                run("act", h, False)

            @block.vector
            def _(h):
                run("dve", h, False)

            @block.gpsimd
            def _(h):
                run("pool", h, False)

            @block.sync
            def _(h):
                run("sp", h, True)
        self.stack.close()


class Rot:
    def __init__(self, P, name, shape, dt, n=2):
        self.bufs = [P.sb(f"{name}{i}", shape, dt) for i in range(n)]
        self.toks = [P.tok(f"{name}{i}") for i in range(n)]
        self.chans = [P.newchan(name) for i in range(n)]
        self.i = 0

    def next(self):
        j = self.i % len(self.bufs)
        self.i += 1
        return self.bufs[j], self.toks[j], self.chans[j]


def _consts_np():
    s = np.arange(128)[:, None]
    c = np.arange(128)[None, :]
    v = np.float32(-1.0 / 16.0)
    cm = np.zeros((4, 128, 128), np.float32)
    cm[0] = np.where(s <= c, v, 0)
    cm[1] = np.where(s >= c, v, 0)
    cm[2] = np.where(s > c, v, 0)
    cm[3] = np.where(s < c, v, 0)
    mask = np.zeros((4, 128, 128), np.float32)
    mask[0] = (s <= c)
    mask[1] = (s >= c)
    mask[2] = (s >= c)
    mask[3] = (s <= c)
    perm = np.zeros((128, 128), np.float32)
    for m in range(128):
        partner = m + 16 if (m % 32) < 16 else m - 16
        perm[partner, m] = 1.0
    ident = np.eye(128, dtype=np.float32)
    return cm, mask, perm, ident


def _rope_tables(pos0, n):
    t = pos0 + np.arange(n)
    row = (t // 64).astype(np.float32)
    col = (t % 64).astype(np.float32)
    inv = (np.float32(10000.0) ** (-(np.arange(16, dtype=np.float32) * np.float32(2.0) / np.float32(32.0)))).astype(np.float32)
    cos = np.zeros((128, n), np.float32)
    sin = np.zeros((128, n), np.float32)
    for p in range(128):
        d = p % 64
        f = d % 16
        pos = row if d < 32 else col
        ang = (pos * inv[f]).astype(np.float32)
        sgn = -1.0 if (d % 32) < 16 else 1.0
        cos[p] = np.cos(ang)
        sin[p] = sgn * np.sin(ang)
    return cos, sin


FUSED_ARENA = 52400
A_LAYER = ("w_ada", "b_ada", "norm1", "w_fm", "w_tm", "up_aug")
B_LAYER = ("w_out", "norm2", "gnorm", "sink", "w_gate", "w_up", "w_down")
B_FROM_A = {"modr": "o_mod", "qs": "o_qs", "ks": "o_ks", "vs": "o_vs", "g": "o_g", "of": "o_of", "ob": "o_ob",
            "qbf": "o_qbf", "qbb": "o_qbb", "st0": "o_st"}


class Ctx:
    def __init__(self):
        self.nc = bass.Bass("TRN2", target_bir_lowering=False)
        self.P = Prog(self.nc)
        self.P.make_banks(6)
        self.ptr = [self.P.ps(f"ptr{i}", [128, 8, 128], BF16) for i in range(2)]
        self.ptr_t = [self.P.tok(excl=True) for i in range(2)]
        self.P.open_scope(FUSED_ARENA)
        self.P.in_scope = False
        self.d = {}

    def ext_in(self, name, shape, dt=F32):
        if name not in self.d:
            self.d[name] = self.nc.dram_tensor(name, list(shape), dt, kind="ExternalInput").ap()
        return self.d[name]

    def internal(self, name, shape, dt=F32):
        if name not in self.d:
            self.d[name] = self.nc.dram_tensor(name, list(shape), dt).ap()
        return self.d[name]

    def inp(self, ph, L, name, shape, dt):
        if name == "x_all":
            return self.ext_in("x_all", shape, dt) if L == 0 else self.d["B0_x_out"]
        if ph == "A" and name in A_LAYER:
            return self.ext_in(f"{name}_{L}", shape, dt)
        if ph == "B" and name in B_LAYER:
            return self.ext_in(f"{name}_{L}", shape, dt)
        if ph == "B" and name in B_FROM_A:
            return self.d[f"A{L}_{B_FROM_A[name]}"]
        return self.ext_in(name, shape, dt)


def build_A(dbg_tiles=None, dbg_scan=True, dbg_lvl=99, dbg_stage=99, ctx=None, L=0):
    fused = ctx is not None
    nc = ctx.nc if fused else bass.Bass("TRN2", target_bir_lowering=False)

    def din(name, shape, dt=F32):
        if fused:
            return ctx.inp("A", L, name, shape, dt)
        return nc.dram_tensor(name, list(shape), dt, kind="ExternalInput").ap()

    def dout(name, shape, dt=F32):
        if fused:
            return ctx.internal(f"A{L}_{name}", shape, dt)
        return nc.dram_tensor(name, list(shape), dt, kind="ExternalOutput").ap()

    x_all = din("x_all", [NT * 128, D])
    cvec = din("cvec", [2, D])
    w_ada = din("w_ada", [D, 6 * D])
    b_ada = din("b_ada", [6 * D])
    norm1 = din("norm1", [D])
    w_fm = din("w_fm", [D, 1280])
    w_tm = din("w_tm", [D, 1440])
    up_aug = din("up_aug", [33, 512])
    cmats = din("cmats", [4, 128, 128])
    masks = din("masks", [4, 128, 128])
    perm = din("perm", [128, 128])
    identd = din("ident", [128, 128])
    ropec = din("ropec", [128, TL])
    ropes = din("ropes", [128, TL])

    o_mod = dout("o_mod", [2, 6 * D])
    o_qs = dout("o_qs", [128, 4, NT * 128], BF16)
    o_ks = dout("o_ks", [128, 2, NT * 128], BF16)
    o_vs = dout("o_vs", [NT * 128, 2, 65], BF16)
    o_g = dout("o_g", [NT * 128, 512], BF16)
    o_of = dout("o_of", [NT * 128, 512])
    o_ob = dout("o_ob", [NT * 128, 512])
    o_qbf = dout("o_qbf", [128, 2, TL], BF16)
    o_qbb = dout("o_qbb", [128, 2, TL], BF16)
    o_st = dout("o_st", [4, 128, 2, 256])
    o_pd = dout("o_pd", [128, 4])

    if fused:
        P, ptr, ptr_t = ctx.P, ctx.ptr, ctx.ptr_t
        P.nchan = 0
        P.open_scope()
        exp_all = ctx.internal(f"exp{L}", [128, 1414], F32)
        exp_f = exp_all[:, 0:1028]
        exp_b = exp_all.bitcast(BF16)[:, 2056:2828]
    else:
        P = Prog(nc)
        P.make_banks(6)
        ptr = [P.ps(f"ptr{i}", [128, 8, 128], BF16) for i in range(2)]
        ptr_t = [P.tok(excl=True) for i in range(2)]

    cm_sb = P.sb("cm_sb", [128, 4, 128], BF16)
    mk_sb = P.sb("mk_sb", [128, 2, 128], F32)
    perm_sb = P.sb("perm_sb", [128, 128], BF16)
    id_f = P.sb("id_f", [128, 128], F32)
    id_b = P.sb("id_b", [128, 128], BF16)
    up_sb = P.sb("up_sb", [33, 512], F32)
    t_const = P.tok("const")
    t_idb = P.tok("idb")
    P.dma("pool", cm_sb[:], cmats.rearrange("m p c -> p m c"), writes=[t_idb], chan="constb")
    P.dma("sp", mk_sb[:], masks[0:2].rearrange("m p c -> p m c"), writes=[t_const], chan="const")
    P.dma("pool", perm_sb[:], perm, writes=[t_idb], chan="constb")
    P.dma("sp", id_f[:], identd, writes=[t_const], chan="const")
    P.dma("sp", up_sb[:], up_aug, writes=[t_const], chan="const")
    P.dma("pool", id_b[:], identd, writes=[t_idb], chan="constb")

    wfm_sb = P.sb("wfm_sb", [128, 8, 1280], BF16)
    wtm_sb = P.sb("wtm_sb", [128, 8, 1440], BF16)
    t_wfm = P.tok("wfm")
    t_wtm = P.tok("wtm")

    if dbg_lvl < 2:
        P.emit()
        return nc
    crow = P.sb("crow", [2, D], F32)
    cTa = P.sb("cTa", [128, 8, 2], F32)
    t_crow = P.tok("crow")
    t_cT = P.tok("cT")
    for k in range(8):
        P.dma("pool", wfm_sb[:, k, :], w_fm[k * 128:(k + 1) * 128, :], writes=[t_wfm], chan="wfm")
    for k in range(8):
        P.dma("pool", wtm_sb[:, k, :], w_tm[k * 128:(k + 1) * 128, :], writes=[t_wtm], chan="wtm")
    P.dma("sp", crow[:], cvec, writes=[t_crow], chan="crow")
    P.op("act", lambda e: e.activation(out=crow[:], in_=crow[:], func=AF.Silu), reads=[t_crow], writes=[t_crow])
    bkc, btc = P.bank()
    for k in range(8):
        P.cont = k > 0
        P.op("pe", lambda e, k=k: e.transpose(out=bkc[:, k * 2:k * 2 + 2], in_=crow[:, k * 128:(k + 1) * 128],
                                              identity=id_f[0:2, 0:2]),
             reads=[t_crow, t_const], writes=[btc])
    P.op("dve", lambda e: e.tensor_copy(out=cTa[:], in_=bkc[:, 0:16].rearrange("p (k j) -> p k j", j=2)),
         reads=[btc], writes=[t_cT])
    mod_sb = P.sb("mod_sb", [2, 2 * D], F32)
    n1_sb = P.sb("n1_sb", [2, D], F32)
    t_mod = P.tok("mod")
    t_n1 = P.tok("n1")
    P.dma("sp", n1_sb[:], norm1.unsqueeze(0).to_broadcast([2, D]), writes=[t_n1], chan="n1")
    wada = Rot(P, "wada", [128, 8, 256], F32, 1)
    bada = Rot(P, "bada", [2, 256], F32, 2)
    mst = Rot(P, "mst", [2, 256], F32, 2)

    mod_pending = {}

    def mod_load(sbk):
        c0 = sbk * 256
        wb, wt, wc = wada.next()
        P.dma("sp", wb[:], w_ada[:, c0:c0 + 256].rearrange("(k p) n -> p k n", p=128), writes=[wt], chan=wc)
        bb_, bbt, bbc = bada.next()
        P.dma("sp", bb_[:], b_ada[c0:c0 + 256].unsqueeze(0).to_broadcast([2, 256]), writes=[bbt], chan=bbc)
        mod_pending[sbk] = (wb, wt, bb_, bbt)

    def mod_compute(sbk):
        c0 = sbk * 256
        wb, wt, bb_, bbt = mod_pending.pop(sbk)
        bk, bt = P.bank()
        for k in range(8):
            P.cont = k > 0
            P.op("pe", lambda e, k=k: e.matmul(out=bk[0:2, 0:256], lhsT=cTa[:, k, :], rhs=wb[:, k, :],
                                               start=(k == 0), stop=(k == 7)),
                 reads=[t_cT, wt], writes=[bt])
        if sbk < 8:
            P.op("dve", lambda e: e.tensor_tensor(out=mod_sb[:, c0:c0 + 256], in0=bk[0:2, 0:256], in1=bb_[:], op=ALU.add),
                 reads=[bt, bbt], writes=[t_mod])
        else:
            mb_, mbt, mbc = mst.next()
            P.op("dve", lambda e: e.tensor_tensor(out=mb_[:], in0=bk[0:2, 0:256], in1=bb_[:], op=ALU.add),
                 reads=[bt, bbt], writes=[mbt])
            P.dma("sp", o_mod[:, c0:c0 + 256], mb_[:], reads=[mbt], chan=mbc)

    for sbk in range(8):
        mod_load(sbk)
        mod_compute(sbk)
    if dbg_lvl < 4:
        P.emit()
        return nc
    mch = P.newchan("omod")
    P.dma("sp", o_mod[:, 0:2 * D], mod_sb[:], reads=[t_mod], chan=mch)
    arow = P.sb("arow", [2, D], F32)
    t_arow = P.tok("arow")
    P.op("dve", lambda e: e.scalar_tensor_tensor(out=arow[:], in0=mod_sb[:, D:2 * D], scalar=1.0, in1=n1_sb[:],
                                                 op0=ALU.add, op1=ALU.mult),
         reads=[t_mod, t_n1], writes=[t_arow])
    if dbg_lvl < 5:
        P.emit()
        return nc
    sel = P.sb("sel", [2, 2, 128], F32)
    t_sel = P.tok("sel")
    P.op("pool", lambda e: e.memset(sel[:], 0.0), writes=[t_sel])
    P.op("pool", lambda e: e.affine_select(out=sel[:], in_=sel[:], pattern=[[-1, 2], [0, 128]],
                                           compare_op=ALU.not_equal, fill=1.0, base=0, channel_multiplier=1),
         reads=[t_sel], writes=[t_sel])
    if dbg_lvl < 6:
        P.emit()
        return nc
    AB = P.sb("AB", [128, 2, D], F32)
    t_AB = P.tok("AB")

    def build_AB(j):
        for which in range(2):
            for hb in range(2):
                bk, bt = P.bank()
                src = arow if which == 0 else mod_sb
                off = hb * 512
                P.op("pe", lambda e, src=src, off=off, bk=bk: e.matmul(out=bk[:], lhsT=sel[:, j, :],
                                                                        rhs=src[:, off:off + 512], start=True, stop=True),
                     reads=[t_sel, t_arow, t_mod], writes=[bt])
                P.op("act", lambda e, which=which, hb=hb, bk=bk: e.copy(out=AB[:, which, hb * 512:(hb + 1) * 512], in_=bk[:]),
                     reads=[bt], writes=[t_AB])

    xt = Rot(P, "xt", [128, D], F32, 2)
    h1 = P.sb("h1", [128, D], F32)
    t_h1 = P.tok("h1")
    hb_ = P.sb("hb", [128, D], BF16)
    t_hb = P.tok("hb")
    hT = Rot(P, "hT", [128, 8, 128], BF16, 2)
    ssq = P.sb("ssq", [128, 2], F32)
    t_ssq = P.tok("ssq")
    zs = P.sb("zs", [128, 32], F32)
    t_zs = P.tok("zs")
    zT = P.sb("zT", [33, 128], F32)
    t_zT = P.tok("zT")
    P.op("pool", lambda e: e.memset(zT[:], 1.0), writes=[t_zT])
    sp_ = P.sb("sp", [128, 512], BF16)
    t_sp = P.tok("sp")
    E = P.sb("E", [128, 4, 128], F32)
    Ei = P.sb("Ei", [128, 4, 128], F32)
    Ek = P.sb("Ek", [128, 512], F32)
    t_E, t_Ei, t_Ek = P.tok("E"), P.tok("Ei"), P.tok("Ek")
    qkg = P.sb("qkg", [128, 4, 128], F32)
    t_qkg = P.tok("qkg")
    ktok = P.sb("ktok", [128, 256], F32)
    t_ktok = P.tok("ktok")
    NF = 2
    qin = [[P.sb(f"qin0_{i}", [128, 2, 128], BF16) for i in range(NF)], [P.sb(f"qin1_{t}", [128, 2, 128], BF16) for t in range(NT)]]
    kin = [[P.sb(f"kin0_{i}", [128, 2, 128], BF16) for i in range(NF)], [P.sb(f"kin1_{t}", [128, 2, 128], BF16) for t in range(NT)]]
    kout = [[P.sb(f"kout0_{i}", [128, 256], BF16) for i in range(NF)], [P.sb(f"kout1_{t}", [128, 256], BF16) for t in range(NT)]]
    t_gl = [[P.tok(f"gl0_{i}") for i in range(NF)], [P.tok(f"gl1_{t}") for t in range(NT)]]
    vb = [P.sb(f"vb{t}", [128, 512], BF16) for t in range(NT)]
    t_vb = [P.tok(f"vb{t}") for t in range(NT)]
    decs = P.sb("decs", [128, NT, 4], F32)
    t_dec = [P.tok(f"dec{t}") for t in range(NT)]

    def gi(d, t):
        return (t % NF) if d == 0 else t

    gs = Rot(P, "gs", [128, 512], BF16, 2)
    qk = P.sb("qk", [128, 6, 128], BF16)
    t_qk = P.tok("qk")
    r1 = P.sb("r1", [128, 6, 128], F32)
    t_r1 = P.tok("r1")
    r2 = P.sb("r2", [128, 6, 128], F32)
    t_r2 = P.tok("r2")
    qko = Rot(P, "qko", [128, 6, 128], BF16, 2)
    vso = Rot(P, "vso", [128, 2, 65], BF16, 2)
    for b_, t_ in zip(vso.bufs, vso.toks):
        P.op("pool", lambda e, b_=b_: e.memset(b_[:], 1.0), writes=[t_])
    rcs = Rot(P, "rcs", [128, 2, 128], F32, 2)

    S = [P.sb(f"S{d}", [128, 2, 256], F32) for d in range(2)]
    Sb = [P.sb(f"Sb{d}", [128, 2, 256], BF16) for d in range(2)]
    cum = [P.sb(f"cum{d}", [128, 2], F32) for d in range(2)]
    t_S = [P.tok(f"S{d}") for d in range(2)]
    t_Sb = [P.tok(f"Sb{d}") for d in range(2)]
    t_cum = [P.tok(f"cum{d}") for d in range(2)]
    ATs = P.sb("ATs", [128, 4, 128], BF16)
    t_ATs = P.tok("ATs")
    ost = Rot(P, "ost", [128, 512], F32, 2)
    qbst = Rot(P, "qbst", [128, 2, 128], BF16, 2)
    stch = P.newchan("st")

    def reset_state(d):
        P.op("pool", lambda e: e.memset(S[d][:], 0.0), writes=[t_S[d]])
        P.op("pool", lambda e: e.memset(Sb[d][:], 0.0), writes=[t_Sb[d]])
        P.op("pool", lambda e: e.memset(cum[d][:], 1.0), writes=[t_cum[d]])

    def scan_step(d, t):
        g = gi(d, t)
        tg_ = t_gl[d][g]
        q_, k_, ko_ = qin[d][g], kin[d][g], kout[d][g]
        bk, bt = P.bank()
        bu, but = P.bank()

        def mm_at(h):
            hh, pr = h % 2, h // 2
            P.op("pe", lambda e: e.matmul(
                out=bk[:, h * 128:(h + 1) * 128], lhsT=k_[hh * 64:(hh + 1) * 64, pr, :],
                rhs=q_[hh * 64:(hh + 1) * 64, pr, :], start=True, stop=True),
                 reads=[tg_], writes=[bt])

        def mm_u(pr):
            P.op("pe", lambda e: e.matmul(out=bu[:, pr * 256:(pr + 1) * 256],
                                          lhsT=ko_[:, pr * 128:(pr + 1) * 128],
                                          rhs=vb[t][:, pr * 256:(pr + 1) * 256], start=True, stop=True),
                 reads=[tg_, t_vb[t]], writes=[but])
        mm_at(0)
        mm_u(0)
        mm_at(1)
        mm_u(1)
        mm_at(2)
        mm_at(3)
        P.op("dve", lambda e: e.tensor_tensor(out=ATs[:], in0=bk[:].rearrange("p (h c) -> p h c", h=4),
                                              in1=mk_sb[:, d, :].unsqueeze(1).to_broadcast([128, 4, 128]), op=ALU.mult),
             reads=[bt, t_const], writes=[t_ATs])
        bo, bot = P.bank()
        for h in range(4):
            hh, pr = h % 2, h // 2
            P.op("pe", lambda e, h=h: e.matmul(out=bo[:, h * 128:(h + 1) * 128], lhsT=ATs[:, h, :],
                                               rhs=vb[t][:, h * 128:(h + 1) * 128], start=True, stop=False),
                 reads=[t_ATs, t_vb[t]], writes=[bot])
            P.op("pe", lambda e, h=h, hh=hh, pr=pr: e.matmul(
                out=bo[:, h * 128:(h + 1) * 128], lhsT=q_[hh * 64:(hh + 1) * 64, pr, :],
                rhs=Sb[d][hh * 64:(hh + 1) * 64, pr, hh * 128:(hh + 1) * 128], start=False, stop=True),
                 reads=[tg_, t_Sb[d]], writes=[bot])
        ob, ot, oc = ost.next()
        P.op("act", lambda e: e.copy(out=ob[:], in_=bo[:]), reads=[bot], writes=[ot])
        dst = o_of if d == 0 else o_ob
        P.dma("sp", dst[t * 128:(t + 1) * 128, :], ob[:], reads=[ot], chan=oc)
        for pr in range(2):
            P.op("dve", lambda e, pr=pr: e.scalar_tensor_tensor(
                out=S[d][:, pr, :], in0=S[d][:, pr, :], scalar=decs[:, t, d * 2 + pr:d * 2 + pr + 1],
                in1=bu[:, pr * 256:(pr + 1) * 256], op0=ALU.mult, op1=ALU.add),
                 reads=[but, t_S[d], t_dec[t]], writes=[t_S[d]])
        P.op("act", lambda e: e.copy(out=Sb[d][:], in_=S[d][:]), reads=[t_S[d]], writes=[t_Sb[d]])
        if t < NT_L:
            qb, qt, qc = qbst.next()
            for pr in range(2):
                P.op("dve", lambda e, pr=pr: e.tensor_scalar(
                    out=qb[:, pr, :], in0=q_[:, pr, :], scalar1=cum[d][:, pr:pr + 1], scalar2=None, op0=ALU.mult),
                     reads=[tg_, t_cum[d]], writes=[qt])
            dstq = o_qbf if d == 0 else o_qbb
            P.dma("sp", dstq[:, :, t * 128:(t + 1) * 128], qb[:], reads=[qt], chan=qc)
            P.op("dve", lambda e: e.tensor_tensor(out=cum[d][:], in0=cum[d][:], in1=decs[:, t, d * 2:d * 2 + 2], op=ALU.mult),
                 reads=[t_cum[d], t_dec[t]], writes=[t_cum[d]])

    def export_state(d, slot):
        P.dma("sp", o_st[slot], S[d][:], reads=[t_S[d]], chan=stch)
        if fused and slot >= 2:
            P.dma("sp", exp_f[:, (slot - 2) * 512:(slot - 1) * 512].rearrange("p (a c) -> p a c", a=2), S[d][:],
                  reads=[t_S[d]], chan=stch)
            P.dma("sp", exp_f[:, 1024 + d * 2:1026 + d * 2], cum[d][:], reads=[t_cum[d]], chan=stch)

    xload = {}

    def load_x(t):
        xb, xtok, xc = xt.next()
        P.dma("sp", xb[:], x_all[t * 128:(t + 1) * 128, :], writes=[xtok], chan=xc)
        rb, rt, rch = None, None, None
        if t < NT_L:
            rb, rt, rch = rcs.next()
            P.dma("sp", rb[:, 0, :], ropec[:, t * 128:(t + 1) * 128], writes=[rt], chan=rch)
            P.dma("sp", rb[:, 1, :], ropes[:, t * 128:(t + 1) * 128], writes=[rt], chan=rch)
        xload[t] = (xb, xtok, rb, rt)

    def prep_tile(t, t_next):
        is_ctx = t >= NT_L
        xb, xtok, rb, rt = xload.pop(t)
        if t_next is not None:
            load_x(t_next)
        P.op("act", lambda e: e.activation(out=hb_[:], in_=xb[:], func=AF.Square, accum_out=ssq[:, 0:1]),
             reads=[xtok], writes=[t_ssq, t_hb])
        P.op("act", lambda e: e.activation(out=ssq[:, 1:2], in_=ssq[:, 0:1], func=AF.Ln, scale=1.0 / D, bias=EPS),
             reads=[t_ssq], writes=[t_ssq])
        P.op("act", lambda e: e.activation(out=ssq[:, 1:2], in_=ssq[:, 1:2], func=AF.Exp, scale=-0.5),
             reads=[t_ssq], writes=[t_ssq])
        P.op("dve", lambda e: e.scalar_tensor_tensor(out=h1[:], in0=xb[:], scalar=ssq[:, 1:2], in1=AB[:, 0, :],
                                                     op0=ALU.mult, op1=ALU.mult),
             reads=[xtok, t_ssq, t_AB], writes=[t_h1])
        P.op("pool", lambda e: e.tensor_tensor(out=hb_[:], in0=h1[:], in1=AB[:, 1, :], op=ALU.add),
             reads=[t_h1, t_AB], writes=[t_hb])
        if dbg_stage <= 1:
            return
        pi = t % 2
        for k in range(8):
            P.cont = k > 0
            P.op("pe", lambda e, k=k: e.transpose(out=ptr[pi][:, k, :], in_=hb_[:, k * 128:(k + 1) * 128], identity=id_b[:]),
                 reads=[t_hb, t_idb], writes=[ptr_t[pi]])
        hTb, hTt, _ = hT.next()
        P.op("act", lambda e: e.copy(out=hTb[:], in_=ptr[pi][:]), reads=[ptr_t[pi]], writes=[hTt])
        if dbg_stage <= 2:
            return
        bA, tA = P.bank()
        bB, tB = P.bank()
        bC, tC = P.bank()
        for cc in (0, 4, 8, 1, 5, 9, 2, 6, 3, 7):
            if cc < 4:
                dst_, tk = bA[:, cc * 128:(cc + 1) * 128], tA
            elif cc < 8:
                dst_, tk = bB[:, (cc - 4) * 128:(cc - 3) * 128], tB
            else:
                dst_, tk = bC[:, (cc - 8) * 128:(cc - 7) * 128], tC
            for k in range(8):
                P.cont = k > 0
                P.op("pe", lambda e, k=k, cc=cc, dst_=dst_: e.matmul(out=dst_, lhsT=wfm_sb[:, k, cc * 128:(cc + 1) * 128],
                                                                      rhs=hTb[:, k, :], start=(k == 0), stop=(k == 7)),
                     reads=[t_wfm, hTt], writes=[tk])
        bV, tV = P.bank()
        bG, tG = P.bank()
        bK, tK = P.bank()
        for (bk, tk, c0, cw) in ((bV, tV, 0, 512), (bG, tG, 512, 512), (bK, tK, 1024, 416)):
            for k in range(8):
                P.cont = k > 0
                P.op("pe", lambda e, k=k, bk=bk, c0=c0, cw=cw: e.matmul(out=bk[:, 0:cw], lhsT=hTb[:, k, :],
                                                                        rhs=wtm_sb[:, k, c0:c0 + cw],
                                                                        start=(k == 0), stop=(k == 7)),
                     reads=[t_wtm, hTt], writes=[tk])
        if dbg_stage <= 3:
            return
        P.op("dve", lambda e: e.tensor_copy(out=qkg[:], in_=bA[:].rearrange("p (a c) -> p a c", a=4)), reads=[tA], writes=[t_qkg])
        if dbg_stage <= 3.1:
            return
        qb_, qt_, qc_ = qko.next()
        if is_ctx:
            P.op("act", lambda e: e.copy(out=qb_[:, 0:4, :], in_=bB[:].rearrange("p (a c) -> p a c", a=4)),
                 reads=[tB], writes=[qt_])
            P.op("act", lambda e: e.copy(out=qb_[:, 4:6, :], in_=bC[:, 0:256].rearrange("p (a c) -> p a c", a=2)),
                 reads=[tC], writes=[qt_])
        else:
            P.op("act", lambda e: e.copy(out=qk[:, 0:4, :], in_=bB[:].rearrange("p (a c) -> p a c", a=4)),
                 reads=[tB], writes=[t_qk])
            P.op("act", lambda e: e.copy(out=qk[:, 4:6, :], in_=bC[:, 0:256].rearrange("p (a c) -> p a c", a=2)),
                 reads=[tC], writes=[t_qk])
        if dbg_stage <= 3.2:
            return
        P.op("act", lambda e: e.copy(out=vb[t][:], in_=bV[:]), reads=[tV], writes=[t_vb[t]])
        if dbg_stage <= 3.3:
            return
        gb, gt, gc = gs.next()
        P.op("dve", lambda e: e.tensor_copy(out=gb[:], in_=bG[:]), reads=[tG], writes=[gt])
        P.dma("sp", o_g[t * 128:(t + 1) * 128, :], gb[:], reads=[gt], chan=gc)
        if dbg_stage <= 3.4:
            return
        vsb, vst, vsc = vso.next()
        P.op("act", lambda e: e.copy(out=vsb[:, :, 0:64], in_=bK[:, 288:416].rearrange("p (j d) -> p j d", j=2)),
             reads=[tK], writes=[vst])
        P.dma("sp", o_vs[t * 128:(t + 1) * 128, :, :], vsb[:], reads=[vst], chan=vsc)
        if dbg_stage <= 3.5:
            return
        P.op("act", lambda e: e.copy(out=zs[:], in_=bK[:, 256:288]), reads=[tK], writes=[t_zs])
        if dbg_stage <= 3.6:
            return
        P.op("dve", lambda e: e.tensor_copy(out=ktok[:], in_=bK[:, 0:256]), reads=[tK], writes=[t_ktok])
        if dbg_stage <= 4:
            return
        bz, tz = P.bank()
        P.op("pe", lambda e: e.transpose(out=bz[0:32, 0:128], in_=zs[:], identity=id_f[:]),
             reads=[t_zs, t_const], writes=[tz])
        P.op("dve", lambda e: e.tensor_copy(out=zT[0:32, :], in_=bz[0:32, 0:128]), reads=[tz], writes=[t_zT])
        bp, tp = P.bank()
        P.op("pe", lambda e: e.matmul(out=bp[:], lhsT=zT[:], rhs=up_sb[:], start=True, stop=True),
             reads=[t_zT, t_const], writes=[tp])
        P.op("act", lambda e: e.activation(out=Ek[:], in_=bp[:], func=AF.Exp, scale=-1.0), reads=[tp], writes=[t_Ek])
        P.op("act", lambda e: e.activation(out=sp_[:], in_=Ek[:], func=AF.Ln, bias=1.0, scale=1.0),
             reads=[t_Ek], writes=[t_sp])
        if dbg_stage <= 5:
            return
        bb, tb = P.bank()
        bg, tg = P.bank()

        def mm_b(ci):
            P.op("pe", lambda e: e.matmul(out=bb[:, ci * 128:(ci + 1) * 128], lhsT=sp_[:, ci * 128:(ci + 1) * 128],
                                          rhs=cm_sb[:, ci // 2, :], start=True, stop=True),
                 reads=[t_sp, t_idb], writes=[tb])

        def mm_g(d):
            P.op("pe", lambda e: e.matmul(out=bg[:, d * 256:(d + 1) * 256], lhsT=cm_sb[:, 2 + d, :],
                                          rhs=sp_[:, d * 256:(d + 1) * 256], start=True, stop=True),
                 reads=[t_sp, t_idb], writes=[tg])
        mm_b(0)
        mm_g(0)
        mm_b(1)
        mm_g(1)
        mm_b(2)
        mm_b(3)
        P.op("act", lambda e: e.activation(out=E[:], in_=bb[:].rearrange("p (a c) -> p a c", a=4), func=AF.Exp),
             reads=[tb], writes=[t_E])
        P.op("act", lambda e: e.activation(out=Ei[:], in_=bb[:].rearrange("p (a c) -> p a c", a=4), func=AF.Exp, scale=-1.0),
             reads=[tb], writes=[t_Ei])
        P.op("act", lambda e: e.activation(out=Ek[:], in_=bg[:], func=AF.Exp), reads=[tg], writes=[t_Ek])
        if dbg_stage <= 6:
            return
        for d in range(2):
            g = gi(d, t)
            P.op("dve", lambda e, d=d, g=g: e.scalar_tensor_tensor(out=qin[d][g][:], in0=qkg[:, 0:2, :], scalar=0.125,
                                                                   in1=E[:, d * 2:d * 2 + 2, :], op0=ALU.mult, op1=ALU.mult),
                 reads=[t_qkg, t_E], writes=[t_gl[d][g]])
            P.op("pool", lambda e, d=d, g=g: e.tensor_tensor(out=kin[d][g][:], in0=qkg[:, 2:4, :], in1=Ei[:, d * 2:d * 2 + 2, :],
                                                             op=ALU.mult),
                 reads=[t_qkg, t_Ei], writes=[t_gl[d][g]])
            P.op("dve", lambda e, d=d, g=g: e.tensor_tensor(out=kout[d][g][:], in0=ktok[:], in1=Ek[:, d * 256:(d + 1) * 256],
                                                            op=ALU.mult),
                 reads=[t_ktok, t_Ek], writes=[t_gl[d][g]])
        P.op("act", lambda e: e.copy(out=decs[:, t, 0:2], in_=E[:, 0:2, 127]), reads=[t_E], writes=[t_dec[t]])
        P.op("act", lambda e: e.copy(out=decs[:, t, 2:4], in_=E[:, 2:4, 0]), reads=[t_E], writes=[t_dec[t]])
        if dbg_stage <= 7:
            return
        if not is_ctx:
            bp1, tp1 = P.bank()
            bp2, tp2 = P.bank()
            P.op("pe", lambda e: e.matmul(out=bp1[:], lhsT=perm_sb[:], rhs=qk[:, 0:4, :].rearrange("p a c -> p (a c)"),
                                          start=True, stop=True), reads=[t_qk, t_idb], writes=[tp1])
            P.op("pe", lambda e: e.matmul(out=bp2[:, 0:256], lhsT=perm_sb[:], rhs=qk[:, 4:6, :].rearrange("p a c -> p (a c)"),
                                          start=True, stop=True), reads=[t_qk, t_idb], writes=[tp2])
            cs = rb[:, 0, :].unsqueeze(1)
            sn = rb[:, 1, :].unsqueeze(1)
            P.op("dve", lambda e: e.tensor_tensor(out=r2[:, 0:4, :], in0=bp1[:].rearrange("p (a c) -> p a c", a=4),
                                                  in1=sn.to_broadcast([128, 4, 128]), op=ALU.mult),
                 reads=[tp1, rt], writes=[t_r2])
            P.op("dve", lambda e: e.tensor_tensor(out=r2[:, 4:6, :], in0=bp2[:, 0:256].rearrange("p (a c) -> p a c", a=2),
                                                  in1=sn.to_broadcast([128, 2, 128]), op=ALU.mult),
                 reads=[tp2, rt], writes=[t_r2])
            P.op("pool", lambda e: e.tensor_tensor(out=r1[:], in0=qk[:], in1=cs.to_broadcast([128, 6, 128]), op=ALU.mult),
                 reads=[t_qk, rt], writes=[t_r1])
            P.op("pool", lambda e: e.tensor_tensor(out=qb_[:], in0=r1[:], in1=r2[:], op=ALU.add),
                 reads=[t_r1, t_r2], writes=[qt_])
        P.dma("sp", o_qs[:, :, t * 128:(t + 1) * 128], qb_[:, 0:4, :], reads=[qt_], chan=qc_)
        P.dma("sp", o_ks[:, :, t * 128:(t + 1) * 128], qb_[:, 4:6, :], reads=[qt_], chan=qc_)
        if fused and t in (0, NT_L - 1):
            side = 0 if t == 0 else 1
            P.dma("sp", exp_b[:, side * 256:(side + 1) * 256].rearrange("p (j c) -> p j c", j=2), qb_[:, 4:6, :],
                  reads=[qt_], chan=qc_)
            P.dma("sp", exp_b[:, 512 + side * 130:512 + (side + 1) * 130].rearrange("p (j d) -> p j d", j=2), vsb[:],
                  reads=[vst], chan=vsc)

    reset_state(0)
    reset_state(1)
    order = [NT_L, NT_L + 1] + list(range(NT_L))
    full = dbg_tiles is None
    if not full:
        order = order[:dbg_tiles]
    if order:
        load_x(order[0])
        build_AB(1)
    for i, t in enumerate(order):
        if t == 0:
            build_AB(0)
        prep_tile(t, order[i + 1] if i + 1 < len(order) else None)
        if t == 0:
            export_state(0, 0)
            reset_state(0)
        if dbg_scan:
            scan_step(0, t)
    if full:
        export_state(0, 2)
        P.dma("sp", o_pd[:, 0:2], cum[0][:], reads=[t_cum[0]], chan=stch)
        mod_load(8)
        for t in [NT_L + 1, NT_L]:
            scan_step(1, t)
        export_state(1, 1)
        reset_state(1)
        for jj, t in enumerate(range(NT_L - 1, -1, -1)):
            mod_compute(8 + jj)
            if jj + 1 < 16:
                mod_load(8 + jj + 1)
            scan_step(1, t)
        export_state(1, 3)
        P.dma("sp", o_pd[:, 2:4], cum[1][:], reads=[t_cum[1]], chan=stch)
    if fused:
        P.close_scope()
        gat_all = ctx.internal(f"gat{L}", [512, 1414], F32)
        rg = [[0, 1, 2, 3], [4, 5, 6, 7]]
        P.op("pool", lambda e: e.collective_compute("AllGather", ALU.bypass, replica_groups=rg,
                                                    ins=[exp_all.opt()], outs=[gat_all.opt()]), chan="cc", inc=1)
        P.barrier()
        return None
    P.emit()
    return nc


def build_B(ctx=None, L=0, last=True):
    fused = ctx is not None
    nc = ctx.nc if fused else bass.Bass("TRN2", target_bir_lowering=False)

    def din(name, shape, dt=F32):
        if fused:
            return ctx.inp("B", L, name, shape, dt)
        return nc.dram_tensor(name, list(shape), dt, kind="ExternalInput").ap()

    def dout(name, shape, dt=F32):
        if fused:
            if name == "y_fin" and last:
                return nc.dram_tensor("y_fin", list(shape), dt, kind="ExternalOutput").ap()
            return ctx.internal(f"B{L}_{name}", shape, dt)
        return nc.dram_tensor(name, list(shape), dt, kind="ExternalOutput").ap()

    x_all = din("x_all", [NT * 128, D])
    modr = din("modr", [2, 6 * D])
    qs_d = din("qs", [128, 4, NT * 128], BF16)
    ks_d = din("ks", [128, 2, NT * 128], BF16)
    vs_d = din("vs", [NT * 128, 2, 65], BF16)
    if not fused:
        kh_d = din("kh", [128, 2, 2, 128], BF16)
        vh_d = din("vh", [2, 128, 2, 65], BF16)
    g_d = din("g", [NT * 128, 512], BF16)
    of_d = din("of", [NT * 128, 512])
    ob_d = din("ob", [NT * 128, 512])
    qbf_d = din("qbf", [128, 2, TL], BF16)
    qbb_d = din("qbb", [128, 2, TL], BF16)
    st0_d = din("st0", [2, 128, 2, 256])
    if fused:
        gat_f = ctx.d[f"gat{L}"].rearrange("(s p) w -> p s w", p=128)[:, :, 0:1028]
        gat_b = ctx.d[f"gat{L}"].bitcast(BF16).rearrange("(s p) w -> p s w", p=128)[:, :, 2056:2828]
        flg_d = din("flags16", [128, 16])
        NFL = 16
    else:
        stF_d = din("stF", [4, 128, 2, 256])
        stB_d = din("stB", [4, 128, 2, 256])
        pd_d = din("pd", [4, 128, 4])
        flg_d = din("flags", [128, 8])
        NFL = 8
    smask_d = din("smask", [4, 128, 128])
    identd = din("ident", [128, 128])
    w_out = din("w_out", [D, D])
    norm2 = din("norm2", [D])
    gnorm = din("gnorm", [512])
    sink = din("sink", [8])
    w_router = din("w_router", [D, NEXP])
    b_router = din("b_router", [NEXP])
    w_gate = din("w_gate", [NEXP, D, DE])
    w_up = din("w_up", [NEXP, D, DE])
    w_down = din("w_down", [NEXP, DE, D])
    fnorm = din("fnorm", [D])

    x_out = dout("x_out", [NT * 128, D])
    y_fin = dout("y_fin", [TL, D])

    if fused:
        P, ptr, ptr_t = ctx.P, ctx.ptr, ctx.ptr_t
        P.nchan = 0
        P.open_scope()
    else:
        P = Prog(nc)
        P.make_banks(6)
        ptr = [P.ps(f"ptr{i}", [128, 8, 128], BF16) for i in range(2)]
        ptr_t = [P.tok(excl=True) for i in range(2)]
        P.open_scope(FUSED_ARENA)

    h2T = P.sb("h2T", [128, 8, NT * 128], BF16)
    t_h2T = [P.tok(f"h2T{t}") for t in range(NT)]
    rw = P.sb("rw", [128, NT, NEXP], F32)
    t_rw = [P.tok(f"rw{t}") for t in range(NT)]
    G2 = P.sb("G2", [128, 2, D], F32)
    fn_sb = P.sb("fn_sb", [128, D], F32)
    id_f = P.sb("id_f", [128, 128], F32)
    id_b = P.sb("id_b", [128, 128], BF16)
    t_c2 = P.tok("c2")
    t_idb = P.tok("idb")
    for j in range(2):
        P.dma("sp", G2[:, j, :], modr[j, 5 * D:6 * D].unsqueeze(0).to_broadcast([128, D]), writes=[t_c2], chan="c2")
    P.dma("sp", fn_sb[:], fnorm.unsqueeze(0).to_broadcast([128, D]), writes=[t_c2], chan="c2")
    P.dma("sp", id_f[:], identd, writes=[t_c2], chan="c2")
    P.dma("pool", id_b[:], identd, writes=[t_idb], chan="c2b")

    persist_end = P.arena_off
    qs = P.sb("qs", [128, 4, NT * 128], BF16)
    ks = P.sb("ks", [128, 2, NT * 128], BF16)
    vs = P.sb("vs", [128, NT, 2, 65], BF16)
    kh = P.sb("kh", [128, 2, 2, 128], BF16)
    vh = P.sb("vh", [128, 2, 2, 65], BF16)
    qbf = P.sb("qbf", [128, 2, TL], BF16)
    qbb = P.sb("qbb", [128, 2, TL], BF16)
    smask = P.sb("smask", [128, 4, 128], BF16)
    wo = P.sb("wo", [128, 8, D], BF16)
    gn_sb = P.sb("gn_sb", [128, 512], F32)
    es_sb = P.sb("es_sb", [128, 8], F32)
    br_sb = P.sb("br_sb", [128, NEXP], F32)
    wr_sb = P.sb("wr_sb", [128, 8, NEXP], F32)
    n2_sb = P.sb("n2_sb", [128, D], F32)
    flg = P.sb("flg", [128, NFL], F32)
    t_in = P.tok("in")
    t_inb = P.tok("inb")
    P.dma("sp", qs[:], qs_d, writes=[t_in], chan="in")
    P.dma("sp", ks[:], ks_d, writes=[t_in], chan="in")
    P.dma("sp", vs[:], vs_d.rearrange("(t p) j d -> p t j d", p=128), writes=[t_in], chan="in")
    if not fused:
        P.dma("sp", kh[:], kh_d, writes=[t_in], chan="in")
        P.dma("sp", vh[:], vh_d.rearrange("s p j d -> p s j d"), writes=[t_in], chan="in")
    P.dma("sp", qbf[:], qbf_d, writes=[t_in], chan="in")
    P.dma("sp", qbb[:], qbb_d, writes=[t_in], chan="in")
    P.dma("sp", gn_sb[:], gnorm.unsqueeze(0).to_broadcast([128, 512]), writes=[t_in], chan="in")
    P.dma("sp", es_sb[:], sink.unsqueeze(0).to_broadcast([128, 8]), writes=[t_in], chan="in")
    P.dma("sp", br_sb[:], b_router.unsqueeze(0).to_broadcast([128, NEXP]), writes=[t_in], chan="in")
    P.dma("sp", wr_sb[:], w_router.rearrange("(k p) e -> p k e", p=128), writes=[t_in], chan="in")
    P.dma("sp", n2_sb[:], norm2.unsqueeze(0).to_broadcast([128, D]), writes=[t_in], chan="in")
    P.dma("sp", flg[:], flg_d, writes=[t_in], chan="in")
    P.dma("pool", smask[:], smask_d.rearrange("m p c -> p m c"), writes=[t_inb], chan="inb")
    for k in range(8):
        P.dma("pool", wo[:, k, :], w_out[k * 128:(k + 1) * 128, :], writes=[t_inb], chan="inb")
    if fused:
        ck = P.sb("ck", [128, 4, 512], BF16)
        cv = P.sb("cv", [128, 4, 260], BF16)
        P.dma("sp", ck[:], gat_b[:, :, 0:512], writes=[t_in], chan="in")
        P.dma("sp", cv[:], gat_b[:, :, 512:772], writes=[t_in], chan="in")
    P.op("act", lambda e: e.activation(out=es_sb[:], in_=es_sb[:], func=AF.Exp), reads=[t_in], writes=[t_in])
    if fused:
        for side in range(2):
            src_half = 1 - side
            for s2 in range(4):
                fcol = flg[:, 8 + side * 4 + s2:9 + side * 4 + s2]
                kin_ = ck[:, s2, src_half * 256:(src_half + 1) * 256].rearrange("p (j c) -> p j c", j=2)
                vin_ = cv[:, s2, src_half * 130:(src_half + 1) * 130].rearrange("p (j d) -> p j d", j=2)
                if s2 == 0:
                    P.op("dve", lambda e, side=side, fcol=fcol, kin_=kin_: e.tensor_scalar(
                        out=kh[:, :, side, :], in0=kin_, scalar1=fcol, scalar2=None, op0=ALU.mult), reads=[t_in], writes=[t_in])
                    P.op("dve", lambda e, side=side, fcol=fcol, vin_=vin_: e.tensor_scalar(
                        out=vh[:, side, :, :], in0=vin_, scalar1=fcol, scalar2=None, op0=ALU.mult), reads=[t_in], writes=[t_in])
                else:
                    P.op("dve", lambda e, side=side, fcol=fcol, kin_=kin_: e.scalar_tensor_tensor(
                        out=kh[:, :, side, :], in0=kin_, scalar=fcol, in1=kh[:, :, side, :], op0=ALU.mult, op1=ALU.add),
                         reads=[t_in], writes=[t_in])
                    P.op("dve", lambda e, side=side, fcol=fcol, vin_=vin_: e.scalar_tensor_tensor(
                        out=vh[:, side, :, :], in0=vin_, scalar=fcol, in1=vh[:, side, :, :], op0=ALU.mult, op1=ALU.add),
                         reads=[t_in], writes=[t_in])

    Sin = [P.sb(f"Sin{d}", [128, 2, 256], F32) for d in range(2)]
    Sinb = [P.sb(f"Sinb{d}", [128, 2, 256], BF16) for d in range(2)]
    t_Sin = [P.tok(f"Sin{d}") for d in range(2)]
    Fst = P.sb("Fst", [128, 2, 4, 2, 256], F32)
    pds = P.sb("pds", [128, 4, 4], F32)
    tmpS = P.sb("tmpS", [128, 256], F32)
    t_tmpS = P.tok("tmpS")
    t_F = P.tok("F")
    P.dma("sp", Sin[0][:], st0_d[0], writes=[t_Sin[0]], chan="st")
    P.dma("sp", Sin[1][:], st0_d[1], writes=[t_Sin[1]], chan="st")
    if fused:
        P.dma("sp", Fst[:, 0].rearrange("p s a c -> p s (a c)"), gat_f[:, :, 0:512], writes=[t_F], chan="st")
        P.dma("sp", Fst[:, 1].rearrange("p s a c -> p s (a c)"), gat_f[:, :, 512:1024], writes=[t_F], chan="st")
        P.dma("sp", pds[:], gat_f[:, :, 1024:1028], writes=[t_F], chan="st")
    else:
        P.dma("sp", Fst[:, 0], stF_d.rearrange("s p a c -> p s a c"), writes=[t_F], chan="st")
        P.dma("sp", Fst[:, 1], stB_d.rearrange("s p a c -> p s a c"), writes=[t_F], chan="st")
        P.dma("sp", pds[:], pd_d.rearrange("s p c -> p s c"), writes=[t_F], chan="st")
    for d in range(2):
        segs = range(4) if d == 0 else range(3, -1, -1)
        for sg in segs:
            for pr in range(2):
                P.op("dve", lambda e, d=d, sg=sg, pr=pr: e.scalar_tensor_tensor(
                    out=tmpS[:], in0=Sin[d][:, pr, :], scalar=pds[:, sg, d * 2 + pr:d * 2 + pr + 1], in1=Fst[:, d, sg, pr, :],
                    op0=ALU.mult, op1=ALU.add), reads=[t_Sin[d], t_F, t_in], writes=[t_tmpS])
                P.op("dve", lambda e, d=d, pr=pr: e.tensor_tensor(out=tmpS[:], in0=tmpS[:], in1=Sin[d][:, pr, :], op=ALU.subtract),
                     reads=[t_Sin[d], t_tmpS], writes=[t_tmpS])
                P.op("dve", lambda e, d=d, sg=sg, pr=pr: e.scalar_tensor_tensor(
                    out=Sin[d][:, pr, :], in0=tmpS[:], scalar=flg[:, d * 4 + sg:d * 4 + sg + 1], in1=Sin[d][:, pr, :],
                    op0=ALU.mult, op1=ALU.add), reads=[t_tmpS, t_in, t_Sin[d]], writes=[t_Sin[d]])
        P.op("act", lambda e, d=d: e.copy(out=Sinb[d][:], in_=Sin[d][:]), reads=[t_Sin[d]], writes=[t_Sin[d]])

    MT = P.sb("MT", [128, 3, D], F32)
    t_MT = P.tok("MT")
    mtch = P.newchan("mt")

    def build_MT(j):
        P.dma("sp", MT[:, 0, :], modr[j, 2 * D:3 * D].unsqueeze(0).to_broadcast([128, D]), writes=[t_MT], chan=mtch)
        P.dma("sp", MT[:, 1, :], modr[j, 4 * D:5 * D].unsqueeze(0).to_broadcast([128, D]), writes=[t_MT], chan=mtch)
        P.dma("sp", MT[:, 2, :], modr[j, 3 * D:4 * D].unsqueeze(0).to_broadcast([128, D]), writes=[t_MT], chan=mtch)
        P.op("dve", lambda e: e.scalar_tensor_tensor(out=MT[:, 1, :], in0=MT[:, 1, :], scalar=1.0, in1=n2_sb[:],
                                                     op0=ALU.add, op1=ALU.mult), reads=[t_MT, t_in], writes=[t_MT])

    xt = Rot(P, "xt", [128, D], F32, 1)
    oft = Rot(P, "oft", [128, 512], F32, 1)
    obt = Rot(P, "obt", [128, 512], F32, 1)
    gt_ = Rot(P, "gt", [128, 512], BF16, 2)
    PT = Rot(P, "PT", [128, 512], BF16, 6)
    mixR = Rot(P, "mix", [128, D], BF16, 2)
    mixbuf = {}

    def get_mix(t):
        if t not in mixbuf:
            mb, mt, _ = mixR.next()
            mixbuf[t] = (mb, mt)
        return mixbuf[t]
    mixT = P.sb("mixT", [128, 8, 128], BF16)
    t_mixT = P.tok("mixT")
    den = P.sb("den", [128, 8], F32)
    t_den = P.tok("den")
    ssq4 = P.sb("ssq4", [128, 8], F32)
    t_ssq4 = P.tok("ssq4")
    og = P.sb("og", [128, 512], F32)
    t_og = P.tok("og")
    sgl = P.sb("sgl", [128, 512], F32)
    t_sgl = P.tok("sgl")
    junk = P.sb("junk", [128, D], BF16)
    t_junk = P.tok("junk")
    x1 = Rot(P, "x1", [128, D], F32, 1)
    ssq = P.sb("ssq", [128, 2], F32)
    t_ssq = P.tok("ssq")
    h2 = P.sb("h2", [128, D], F32)
    t_h2 = P.tok("h2")
    h2Tf = P.sb("h2Tf", [128, 8, 128], F32)
    t_h2Tf = P.tok("h2Tf")
    rt_ = P.sb("rt", [128, 8, NEXP], F32)
    t_rt = P.tok("rt")

    def swa_tile(t):
        is_ctx = t >= NT_L
        mix, t_mix = get_mix(t)
        if is_ctx:
            keys = [("k", NT_L, None), ("k", NT_L + 1, None)]
        else:
            keys = []
            if t == 0:
                keys.append(("h", 0, 2))
            else:
                keys.append(("k", t - 1, 0))
            keys.append(("k", t, None))
            if t == NT_L - 1:
                keys.append(("h", 1, 3))
            else:
                keys.append(("k", t + 1, 1))
            keys += [("k", NT_L, None), ("k", NT_L + 1, None)]
        for j in range(2):
            pts = []
            for k0 in range(0, len(keys), 2):
                grp = keys[k0:k0 + 2]
                banks_ = [P.bank() for _ in grp]
                for g in range(4):
                    half, pair = g % 2, 2 * j + g // 2
                    for (kind, idx, mi), (bs, bst) in zip(grp, banks_):
                        if kind == "k":
                            kap = ks[half * 64:(half + 1) * 64, j, idx * 128:(idx + 1) * 128]
                        else:
                            kap = kh[half * 64:(half + 1) * 64, j, idx, :]
                        P.op("pe", lambda e, g=g, kap=kap, half=half, pair=pair, bs=bs: e.matmul(
                            out=bs[:, g * 128:(g + 1) * 128], lhsT=kap, rhs=qs[half * 64:(half + 1) * 64, pair, t * 128:(t + 1) * 128],
                            start=True, stop=True), reads=[t_in], writes=[bst])
                for (kind, idx, mi), (bs, bst) in zip(grp, banks_):
                    pb, pt_, _ = PT.next()
                    P.op("act", lambda e, pb=pb, bs=bs: e.activation(out=pb[:], in_=bs[:], func=AF.Exp, scale=0.125),
                         reads=[bst], writes=[pt_])
                    if mi is not None:
                        P.op("pool", lambda e, pb=pb, mi=mi: e.tensor_tensor(
                            out=pb[:].rearrange("p (g c) -> p g c", g=4), in0=pb[:].rearrange("p (g c) -> p g c", g=4),
                            in1=smask[:, mi, :].unsqueeze(1).to_broadcast([128, 4, 128]), op=ALU.mult),
                             reads=[pt_, t_inb], writes=[pt_])
                    pts.append((pb, pt_, kind, idx))
            bo, bot = P.bank()
            for g in range(4):
                for ki, (pb, pt_, kind, idx) in enumerate(pts):
                    vap = vs[:, idx, j, :] if kind == "k" else vh[:, idx, j, :]
                    P.op("pe", lambda e, g=g, vap=vap, pb=pb, bo=bo, ki=ki: e.matmul(
                        out=bo[:, g * 65:(g + 1) * 65], lhsT=pb[:, g * 128:(g + 1) * 128], rhs=vap,
                        start=(ki == 0), stop=(ki == len(pts) - 1)), reads=[pt_, t_in], writes=[bot])
            bo3 = bo[:, 0:260].rearrange("p (g c) -> p g c", g=4)
            P.op("dve", lambda e, bo3=bo3, j=j: e.tensor_tensor(out=den[:, j * 4:(j + 1) * 4], in0=bo3[:, :, 64],
                                                                in1=es_sb[:, j * 4:(j + 1) * 4], op=ALU.add),
                 reads=[bot, t_in], writes=[t_den])
            P.op("dve", lambda e, j=j: e.reciprocal(out=den[:, j * 4:(j + 1) * 4], in_=den[:, j * 4:(j + 1) * 4]),
                 reads=[t_den], writes=[t_den])
            P.op("dve", lambda e, bo3=bo3, j=j: e.tensor_tensor(
                out=mix[:, 512 + j * 256:512 + (j + 1) * 256].rearrange("p (g c) -> p g c", g=4), in0=bo3[:, :, 0:64],
                in1=den[:, j * 4:(j + 1) * 4].unsqueeze(2).to_broadcast([128, 4, 64]), op=ALU.mult),
                 reads=[bot, t_den], writes=[t_mix])

    def gla_tile(t):
        is_ctx = t >= NT_L
        mix, t_mix = get_mix(t)
        ofb, oft_t, ofc = oft.next()
        obb, obt_t, obc = obt.next()
        gb, gt_t, gc = gt_.next()
        P.dma("sp", ofb[:], of_d[t * 128:(t + 1) * 128, :], writes=[oft_t], chan=ofc)
        P.dma("sp", obb[:], ob_d[t * 128:(t + 1) * 128, :], writes=[obt_t], chan=obc)
        P.dma("sp", gb[:], g_d[t * 128:(t + 1) * 128, :], writes=[gt_t], chan=gc)
        P.op("pool", lambda e: e.tensor_tensor(out=og[:], in0=ofb[:], in1=obb[:], op=ALU.add),
             reads=[oft_t, obt_t], writes=[t_og])
        if not is_ctx:
            bf, bft = P.bank()
            for h in range(4):
                hh, pr = h % 2, h // 2
                P.op("pe", lambda e, h=h, hh=hh, pr=pr: e.matmul(
                    out=bf[:, h * 128:(h + 1) * 128], lhsT=qbf[hh * 64:(hh + 1) * 64, pr, t * 128:(t + 1) * 128],
                    rhs=Sinb[0][hh * 64:(hh + 1) * 64, pr, hh * 128:(hh + 1) * 128], start=True, stop=False),
                     reads=[t_in, t_Sin[0]], writes=[bft])
                P.op("pe", lambda e, h=h, hh=hh, pr=pr: e.matmul(
                    out=bf[:, h * 128:(h + 1) * 128], lhsT=qbb[hh * 64:(hh + 1) * 64, pr, t * 128:(t + 1) * 128],
                    rhs=Sinb[1][hh * 64:(hh + 1) * 64, pr, hh * 128:(hh + 1) * 128], start=False, stop=True),
                     reads=[t_in, t_Sin[1]], writes=[bft])
            P.op("dve", lambda e: e.tensor_tensor(out=og[:], in0=og[:], in1=bf[:], op=ALU.add), reads=[t_og, bft], writes=[t_og])
        for h in range(4):
            P.op("act", lambda e, h=h: e.activation(out=junk[:, h * 128:(h + 1) * 128], in_=og[:, h * 128:(h + 1) * 128],
                                                    func=AF.Square, accum_out=ssq4[:, h:h + 1]),
                 reads=[t_og], writes=[t_ssq4, t_junk])
        P.op("act", lambda e: e.activation(out=ssq4[:, 4:8], in_=ssq4[:, 0:4], func=AF.Ln, scale=1.0 / 128, bias=EPS),
             reads=[t_ssq4], writes=[t_ssq4])
        P.op("act", lambda e: e.activation(out=ssq4[:, 4:8], in_=ssq4[:, 4:8], func=AF.Exp, scale=-0.5),
             reads=[t_ssq4], writes=[t_ssq4])
        P.op("act", lambda e: e.activation(out=sgl[:], in_=gb[:], func=AF.Silu), reads=[gt_t], writes=[t_sgl])
        P.op("dve", lambda e: e.tensor_tensor(out=og[:].rearrange("p (h c) -> p h c", h=4),
                                              in0=og[:].rearrange("p (h c) -> p h c", h=4),
                                              in1=ssq4[:, 4:8].unsqueeze(2).to_broadcast([128, 4, 128]), op=ALU.mult),
             reads=[t_og, t_ssq4], writes=[t_og])
        P.op("pool", lambda e: e.tensor_tensor(out=og[:], in0=og[:], in1=gn_sb[:], op=ALU.mult), reads=[t_og, t_in], writes=[t_og])
        P.op("dve", lambda e: e.tensor_tensor(out=mix[:, 0:512], in0=og[:], in1=sgl[:], op=ALU.mult),
             reads=[t_og, t_sgl], writes=[t_mix])

    def post_tile(t):
        is_ctx = t >= NT_L
        mix, t_mix = get_mix(t)
        j = 1 if is_ctx else 0
        xb, xtok, xc = xt.next()
        P.dma("sp", xb[:], x_all[t * 128:(t + 1) * 128, :], writes=[xtok], chan=xc)
        pi = t % 2
        for k in range(8):
            P.cont = k > 0
            P.op("pe", lambda e, k=k: e.transpose(out=ptr[pi][:, k, :], in_=mix[:, k * 128:(k + 1) * 128], identity=id_b[:]),
                 reads=[t_mix, t_idb], writes=[ptr_t[pi]])
        P.op("act", lambda e: e.copy(out=mixT[:], in_=ptr[pi][:]), reads=[ptr_t[pi]], writes=[t_mixT])
        x1b, x1t, x1c = x1.next()
        for cb in range(2):
            bk, bt = P.bank()
            for k in range(8):
                P.cont = k > 0
                P.op("pe", lambda e, k=k, cb=cb, bk=bk: e.matmul(out=bk[:], lhsT=mixT[:, k, :], rhs=wo[:, k, cb * 512:(cb + 1) * 512],
                                                                 start=(k == 0), stop=(k == 7)),
                     reads=[t_mixT, t_inb], writes=[bt])
            P.op("dve", lambda e, cb=cb, bk=bk: e.tensor_tensor(out=x1b[:, cb * 512:(cb + 1) * 512], in0=bk[:],
                                                                in1=MT[:, 0, cb * 512:(cb + 1) * 512], op=ALU.mult),
                 reads=[bt, t_MT], writes=[x1t])
        P.op("pool", lambda e: e.tensor_tensor(out=x1b[:], in0=x1b[:], in1=xb[:], op=ALU.add), reads=[x1t, xtok], writes=[x1t])
        P.dma("sp", x_out[t * 128:(t + 1) * 128, :], x1b[:], reads=[x1t], chan=x1c)
        P.op("act", lambda e: e.activation(out=h2[:], in_=x1b[:], func=AF.Square, accum_out=ssq[:, 0:1]),
             reads=[x1t], writes=[t_ssq, t_h2])
        P.op("act", lambda e: e.activation(out=ssq[:, 1:2], in_=ssq[:, 0:1], func=AF.Ln, scale=1.0 / D, bias=EPS),
             reads=[t_ssq], writes=[t_ssq])
        P.op("act", lambda e: e.activation(out=ssq[:, 1:2], in_=ssq[:, 1:2], func=AF.Exp, scale=-0.5),
             reads=[t_ssq], writes=[t_ssq])
        P.op("dve", lambda e: e.scalar_tensor_tensor(out=h2[:], in0=x1b[:], scalar=ssq[:, 1:2], in1=MT[:, 1, :],
                                                     op0=ALU.mult, op1=ALU.mult), reads=[x1t, t_ssq, t_MT], writes=[t_h2])
        P.op("pool", lambda e: e.tensor_tensor(out=h2[:], in0=h2[:], in1=MT[:, 2, :], op=ALU.add), reads=[t_h2, t_MT], writes=[t_h2])
        for hb in range(2):
            bk, bt = P.bank()
            for k4 in range(4):
                k = hb * 4 + k4
                P.cont = k > 0
                P.op("pe", lambda e, k=k, k4=k4, bk=bk: e.transpose(out=bk[:, k4 * 128:(k4 + 1) * 128], in_=h2[:, k * 128:(k + 1) * 128],
                                                                    identity=id_f[:]), reads=[t_h2, t_c2], writes=[bt])
            P.op("act", lambda e, hb=hb, bk=bk: e.copy(out=h2Tf[:, hb * 4:(hb + 1) * 4, :], in_=bk[:].rearrange("p (a c) -> p a c", a=4)),
                 reads=[bt], writes=[t_h2Tf])
            P.op("dve", lambda e, hb=hb, bk=bk: e.tensor_copy(out=h2T[:, hb * 4:(hb + 1) * 4, t * 128:(t + 1) * 128],
                                                              in_=bk[:].rearrange("p (a c) -> p a c", a=4)),
                 reads=[bt], writes=[t_h2T[t]])
        bl, blt = P.bank()
        for k in range(8):
            P.cont = k > 0
            P.op("pe", lambda e, k=k: e.matmul(out=bl[:, 0:NEXP], lhsT=h2Tf[:, k, :], rhs=wr_sb[:, k, :], start=(k == 0), stop=(k == 7)),
                 reads=[t_h2Tf, t_in], writes=[blt])
        S_, SB, T1, T2, M1, M2 = (rt_[:, i, :] for i in range(6))
        R = rw[:, t, :]
        tk = [t_rt]

        def dv(fn, extra_r=(), extra_w=()):
            P.op("dve", fn, reads=tk + list(extra_r), writes=tk + list(extra_w))
        P.op("act", lambda e: e.activation(out=T1, in_=bl[:, 0:NEXP], func=AF.Exp, scale=-1.0), reads=[blt], writes=tk)
        dv(lambda e: e.tensor_scalar(out=T1, in0=T1, scalar1=1.0, scalar2=None, op0=ALU.add))
        dv(lambda e: e.reciprocal(out=S_, in_=T1))
        dv(lambda e: e.tensor_tensor(out=SB, in0=S_, in1=br_sb[:], op=ALU.add), [t_in])
        sb3 = SB.rearrange("p (g c) -> p g c", g=4)
        t13 = T1.rearrange("p (g c) -> p g c", g=4)
        t23 = T2.rearrange("p (g c) -> p g c", g=4)
        dv(lambda e: e.tensor_reduce(out=M1[:, 0:4], in_=sb3, axis=AX.X, op=ALU.max))
        dv(lambda e: e.tensor_tensor(out=t13, in0=sb3, in1=M1[:, 0:4].unsqueeze(2).to_broadcast([128, 4, 4]), op=ALU.is_equal))
        dv(lambda e: e.scalar_tensor_tensor(out=T2, in0=T1, scalar=-1e9, in1=SB, op0=ALU.mult, op1=ALU.add))
        dv(lambda e: e.tensor_reduce(out=M1[:, 4:8], in_=t23, axis=AX.X, op=ALU.max))
        dv(lambda e: e.tensor_tensor(out=M1[:, 8:12], in0=M1[:, 0:4], in1=M1[:, 4:8], op=ALU.add))
        dv(lambda e: e.tensor_reduce(out=M1[:, 12:13], in_=M1[:, 8:12], axis=AX.X, op=ALU.max))
        dv(lambda e: e.tensor_scalar(out=M2[:, 0:4], in0=M1[:, 8:12], scalar1=M1[:, 12:13], scalar2=None, op0=ALU.is_equal))
        dv(lambda e: e.tensor_scalar(out=M2[:, 0:4], in0=M2[:, 0:4], scalar1=-1.0, scalar2=1e9, op0=ALU.add, op1=ALU.mult))
        dv(lambda e: e.tensor_tensor(out=t13, in0=sb3, in1=M2[:, 0:4].unsqueeze(2).to_broadcast([128, 4, 4]), op=ALU.add))
        dv(lambda e: e.tensor_reduce(out=M2[:, 4:5], in_=T1, axis=AX.X, op=ALU.max))
        dv(lambda e: e.tensor_scalar(out=T2, in0=T1, scalar1=M2[:, 4:5], scalar2=None, op0=ALU.is_equal))
        dv(lambda e: e.scalar_tensor_tensor(out=T1, in0=T2, scalar=-1e9, in1=T1, op0=ALU.mult, op1=ALU.add))
        dv(lambda e: e.tensor_reduce(out=M2[:, 5:6], in_=T1, axis=AX.X, op=ALU.max))
        dv(lambda e: e.tensor_scalar(out=T1, in0=T1, scalar1=M2[:, 5:6], scalar2=None, op0=ALU.is_equal))
        dv(lambda e: e.tensor_tensor(out=T1, in0=T1, in1=T2, op=ALU.add))
        dv(lambda e: e.tensor_tensor(out=T1, in0=T1, in1=S_, op=ALU.mult))
        dv(lambda e: e.tensor_reduce(out=M2[:, 6:7], in_=T1, axis=AX.X, op=ALU.add))
        dv(lambda e: e.reciprocal(out=M2[:, 6:7], in_=M2[:, 6:7]))
        dv(lambda e: e.tensor_scalar(out=R, in0=T1, scalar1=M2[:, 6:7], scalar2=None, op0=ALU.mult), (), [t_rw[t]])

    do_ctx = not (fused and last)
    tiles = ([NT_L, NT_L + 1] if do_ctx else []) + list(range(NT_L))
    if do_ctx:
        build_MT(1)
    swa_tile(tiles[0])
    gla_tile(tiles[0])
    for i, t in enumerate(tiles):
        if i + 1 < len(tiles):
            swa_tile(tiles[i + 1])
            gla_tile(tiles[i + 1])
        if t == 0:
            build_MT(0)
        post_tile(t)
    P.close_scope()

    P.open_scope()
    P.arena_off = persist_end
    acc = P.sb("acc", [128, NT, D], F32)
    t_acc = [P.tok(f"acc{t}") for t in range(NT)]
    wg = Rot(P, "wg", [128, 8, DE], BF16, 2)
    wu = Rot(P, "wu", [128, 8, DE], BF16, 2)
    wd = Rot(P, "wd", [128, 4, D], BF16, 2)
    sg = Rot(P, "sg", [128, 512], F32, 2)
    hid = Rot(P, "hid", [128, 4, 512], BF16, 2)
    hid_toks = [[P.tok(f"hid{i}_{fc}") for fc in range(4)] for i in range(2)]
    groups = [(g * 4, 4) for g in range(4)] + ([(NT_L, 2)] if do_ctx else [])
    xr = Rot(P, "xr", [128, D], F32, 2)
    yo = Rot(P, "yo", [128, D], F32, 2)
    ssq_f = P.sb("ssq_f", [128, 2], F32)
    t_ssqf = P.tok("ssqf")
    junk2 = P.sb("junk2", [128, D], BF16)

    def final_tile(t):
        j = 1 if t >= NT_L else 0
        xb, xtok, xc = xr.next()
        P.dma("sp", xb[:], x_out[t * 128:(t + 1) * 128, :], writes=[xtok], chan=xc)
        P.op("pool", lambda e: e.tensor_tensor(out=acc[:, t, :], in0=acc[:, t, :], in1=G2[:, j, :], op=ALU.mult),
             reads=[t_acc[t], t_c2], writes=[t_acc[t]])
        P.op("dve", lambda e: e.tensor_tensor(out=xb[:], in0=xb[:], in1=acc[:, t, :], op=ALU.add),
             reads=[t_acc[t], xtok], writes=[xtok])
        if t < NT_L and last:
            P.op("act", lambda e: e.activation(out=junk2[:], in_=xb[:], func=AF.Square, accum_out=ssq_f[:, 0:1]),
                 reads=[xtok], writes=[t_ssqf])
            P.op("act", lambda e: e.activation(out=ssq_f[:, 1:2], in_=ssq_f[:, 0:1], func=AF.Ln, scale=1.0 / D, bias=EPS),
                 reads=[t_ssqf], writes=[t_ssqf])
            P.op("act", lambda e: e.activation(out=ssq_f[:, 1:2], in_=ssq_f[:, 1:2], func=AF.Exp, scale=-0.5),
                 reads=[t_ssqf], writes=[t_ssqf])
            yb, yt, yc = yo.next()
            P.op("dve", lambda e: e.scalar_tensor_tensor(out=yb[:], in0=xb[:], scalar=ssq_f[:, 1:2], in1=fn_sb[:],
                                                         op0=ALU.mult, op1=ALU.mult),
                 reads=[xtok, t_ssqf, t_c2], writes=[yt])
            P.dma("sp", y_fin[t * 128:(t + 1) * 128, :], yb[:], reads=[yt], chan=yc)
        P.dma("sp", x_out[t * 128:(t + 1) * 128, :], xb[:], reads=[xtok], chan=xc)

    wbufs = {}
    jstate = {}

    def load_expert(ex):
        wgb, wgt, wgc = wg.next()
        wub, wut, wuc = wu.next()
        wdb, wdt, wdc = wd.next()
        for k in range(8):
            P.dma("pool", wgb[:, k, :], w_gate[ex, k * 128:(k + 1) * 128, :], writes=[wgt], chan=wgc)
        for k in range(8):
            P.dma("pool", wub[:, k, :], w_up[ex, k * 128:(k + 1) * 128, :], writes=[wut], chan=wuc)
        for k in range(4):
            P.dma("pool", wdb[:, k, :], w_down[ex, k * 128:(k + 1) * 128, :], writes=[wdt], chan=wdc)
        wbufs[ex] = (wgb, wgt, wub, wut, wdb, wdt)

    def moe_gu(job, fc):
        ex, t0, ntl = job
        wgb, wgt, wub, wut, wdb, wdt = wbufs[ex]
        n = ntl * 128
        if fc == 0:
            hb0, _, _ = hid.next()
            jstate[job] = (hb0, hid_toks[(hid.i - 1) % 2])
        hb_, htk = jstate[job]
        bg_, bgt = P.bank()
        bu_, but = P.bank()
        for k in range(8):
            P.cont = k > 0
            P.op("pe", lambda e, k=k, fc=fc, bg_=bg_: e.matmul(out=bg_[:, 0:n], lhsT=wgb[:, k, fc * 128:(fc + 1) * 128],
                                                               rhs=h2T[:, k, t0 * 128:t0 * 128 + n], start=(k == 0), stop=(k == 7)),
                 reads=[wgt] + t_h2T[t0:t0 + ntl], writes=[bgt])
            P.cont = k > 0
            P.op("pe", lambda e, k=k, fc=fc, bu_=bu_: e.matmul(out=bu_[:, 0:n], lhsT=wub[:, k, fc * 128:(fc + 1) * 128],
                                                               rhs=h2T[:, k, t0 * 128:t0 * 128 + n], start=(k == 0), stop=(k == 7)),
                 reads=[wut] + t_h2T[t0:t0 + ntl], writes=[but])
        sgb, sgt, _ = sg.next()
        P.op("act", lambda e, sgb=sgb, bg_=bg_: e.activation(out=sgb[:, 0:n], in_=bg_[:, 0:n], func=AF.Silu),
             reads=[bgt], writes=[sgt])
        P.op("dve", lambda e, sgb=sgb, bu_=bu_, fc=fc: e.tensor_tensor(out=hb_[:, fc, 0:n], in0=sgb[:, 0:n], in1=bu_[:, 0:n],
                                                                       op=ALU.mult),
             reads=[sgt, but], writes=[htk[fc]])

    def moe_down(job):
        ex, t0, ntl = job
        wgb, wgt, wub, wut, wdb, wdt = wbufs[ex]
        hb_, htk = jstate[job]
        for tt in range(ntl):
            t = t0 + tt
            bys = [P.bank(), P.bank()]
            for fc in range(4):
                for cb in range(2):
                    by, byt = bys[cb]
                    P.cont = fc > 0
                    P.op("pe", lambda e, fc=fc, cb=cb, by=by, tt=tt: e.matmul(
                        out=by[:], lhsT=hb_[:, fc, tt * 128:(tt + 1) * 128], rhs=wdb[:, fc, cb * 512:(cb + 1) * 512],
                        start=(fc == 0), stop=(fc == 3)), reads=[htk[fc], wdt], writes=[byt])
            for cb in range(2):
                by, byt = bys[cb]
                if ex == 0:
                    P.op("dve", lambda e, cb=cb, by=by, t=t: e.tensor_scalar(
                        out=acc[:, t, cb * 512:(cb + 1) * 512], in0=by[:], scalar1=rw[:, t, 0:1], scalar2=None, op0=ALU.mult),
                         reads=[byt, t_rw[t]], writes=[t_acc[t]])
                else:
                    P.op("dve", lambda e, cb=cb, by=by, t=t: e.scalar_tensor_tensor(
                        out=acc[:, t, cb * 512:(cb + 1) * 512], in0=by[:], scalar=rw[:, t, ex:ex + 1],
                        in1=acc[:, t, cb * 512:(cb + 1) * 512], op0=ALU.mult, op1=ALU.add),
                         reads=[byt, t_rw[t], t_acc[t]], writes=[t_acc[t]])
            if ex == NEXP - 1:
                final_tile(t)

    jobs = [(ex, t0, ntl) for ex in range(NEXP) for (t0, ntl) in groups]
    load_expert(0)
    for fc in range(4):
        moe_gu(jobs[0], fc)
    for i, job in enumerate(jobs):
        nj = jobs[i + 1] if i + 1 < len(jobs) else None
        if nj is not None:
            if nj[0] != job[0]:
                load_expert(nj[0])
            moe_gu(nj, 0)
        moe_down(job)
        if nj is not None:
            for fc in range(1, 4):
                moe_gu(nj, fc)

    if fused:
        P.close_scope()
        return None
    P.emit()
    return nc


IN_OFF = {"lq": (0, 256), "lk": (256, 512), "lv": (512, 1024), "lg": (1024, 1536), "lzf": (1536, 1552),
          "lzb": (1552, 1568), "lsq": (1568, 2080), "lsk": (2080, 2208), "lsv": (2208, 2336)}
_CACHE = {}


def _get(name, fn):
    if name not in _CACHE:
        _CACHE[name] = fn()
    return _CACHE[name]


def _sl(w, key):
    a, b = IN_OFF[key]
    return w[:, a:b]


def host_A_inputs(i, xl, xc, inp):
    w_in = inp["w_in"][i]
    lsk = _sl(w_in, "lsk")
    w_fm = np.ascontiguousarray(np.concatenate(
        [_sl(w_in, "lq"), _sl(w_in, "lk"), _sl(w_in, "lsq"), lsk[:, 0:64], lsk[:, 0:64], lsk[:, 64:128], lsk[:, 64:128]], axis=1))
    w_tm = np.ascontiguousarray(np.concatenate(
        [_sl(w_in, "lv"), _sl(w_in, "lg"), _sl(w_in, "lk"), _sl(w_in, "lzf"), _sl(w_in, "lzb"), _sl(w_in, "lsv")], axis=1))
    up_aug = np.zeros((33, 512), np.float32)
    up_aug[0:16, 0:256] = inp["gla_up_f"][i]
    up_aug[16:32, 256:512] = inp["gla_up_b"][i]
    up_aug[32, 0:256] = inp["gla_bias_f"][i]
    up_aug[32, 256:512] = inp["gla_bias_b"][i]
    cm, mask, perm, ident = _consts_np()
    maps = []
    for r in range(8):
        b, seg = r // 4, r % 4
        cos, sin = _rope_tables(seg * TL, TL)
        maps.append({
            "x_all": np.ascontiguousarray(np.concatenate([xl[b, seg * TL:(seg + 1) * TL], xc[b]], axis=0)),
            "cvec": np.ascontiguousarray(np.stack([inp["c"][b], inp["c_ctx"]], axis=0)),
            "w_ada": inp["w_ada"][i], "b_ada": inp["b_ada"][i], "norm1": inp["norm1"][i],
            "w_fm": w_fm, "w_tm": w_tm, "up_aug": up_aug, "cmats": cm, "masks": mask, "perm": perm, "ident": ident,
            "ropec": cos, "ropes": sin,
        })
    return maps


def run_A(i, xl, xc, inp):
    nc = _get("A", build_A)
    res = run_bass_kernel_spmd(nc, host_A_inputs(i, xl, xc, inp), core_ids=list(range(8)))
    return res.results


def host_B_inputs(i, mapsA, resA, inp):
    cm, mask, perm, ident = _consts_np()
    maps = []
    zk = np.zeros((128, 2, 128), NPBF)
    zv = np.zeros((128, 2, 65), NPBF)
    for r in range(8):
        b, seg = r // 4, r % 4
        ra = resA[r]
        L0 = (NT_L - 1) * 128
        kl = np.asarray(resA[r - 1]["o_ks"])[:, :, L0:L0 + 128] if seg > 0 else zk
        kr = np.asarray(resA[r + 1]["o_ks"])[:, :, 0:128] if seg < 3 else zk
        vl = np.asarray(resA[r - 1]["o_vs"])[L0:L0 + 128] if seg > 0 else zv
        vr = np.asarray(resA[r + 1]["o_vs"])[0:128] if seg < 3 else zv
        flags = np.zeros((128, 8), np.float32)
        for s2 in range(4):
            flags[:, s2] = 1.0 if s2 < seg else 0.0
            flags[:, 4 + s2] = 1.0 if s2 > seg else 0.0
        smask = np.stack([mask[2], mask[3], mask[2] if seg > 0 else 0 * mask[2], mask[3] if seg < 3 else 0 * mask[3]], axis=0)
        maps.append({
            "x_all": mapsA[r]["x_all"], "modr": np.asarray(ra["o_mod"]),
            "qs": np.asarray(ra["o_qs"]), "ks": np.asarray(ra["o_ks"]), "vs": np.asarray(ra["o_vs"]),
            "kh": np.ascontiguousarray(np.stack([kl, kr], axis=2)), "vh": np.ascontiguousarray(np.stack([vl, vr], axis=0)),
            "g": np.asarray(ra["o_g"]), "of": np.asarray(ra["o_of"]), "ob": np.asarray(ra["o_ob"]),
            "qbf": np.asarray(ra["o_qbf"]), "qbb": np.asarray(ra["o_qbb"]),
            "st0": np.ascontiguousarray(np.asarray(ra["o_st"])[0:2]),
            "stF": np.ascontiguousarray(np.stack([np.asarray(resA[b * 4 + s2]["o_st"])[2] for s2 in range(4)], axis=0)),
            "stB": np.ascontiguousarray(np.stack([np.asarray(resA[b * 4 + s2]["o_st"])[3] for s2 in range(4)], axis=0)),
            "pd": np.ascontiguousarray(np.stack([np.asarray(resA[b * 4 + s2]["o_pd"]) for s2 in range(4)], axis=0)),
            "flags": flags, "smask": np.ascontiguousarray(smask.astype(np.float32)), "ident": ident,
            "w_out": inp["w_out"][i], "norm2": inp["norm2"][i], "gnorm": inp["gla_norm"][i], "sink": inp["swa_sink"][i],
            "w_router": inp["w_router"], "b_router": inp["b_router"],
            "w_gate": inp["w_gate"][i], "w_up": inp["w_up"][i], "w_down": inp["w_down"][i], "fnorm": inp["final_norm"],
        })
    return maps


def run_layer(i, xl, xc, inp):
    mapsA = host_A_inputs(i, xl, xc, inp)
    resA = run_bass_kernel_spmd(_get("A", build_A), mapsA, core_ids=list(range(8))).results
    mapsB = host_B_inputs(i, mapsA, resA, inp)
    resB = run_bass_kernel_spmd(_get("B", build_B), mapsB, core_ids=list(range(8))).results
    xo = np.stack([np.asarray(r["x_out"]) for r in resB], axis=0)
    xl2 = xo[:, :TL].reshape(2, 4 * TL, D)
    xc2 = xo[[0, 4], TL:]
    y = np.stack([np.asarray(r["y_fin"]) for r in resB], axis=0).reshape(2, 4 * TL, D)
    return xl2, xc2, y


def build_fused():
    ctx = Ctx()
    for L in range(2):
        build_A(ctx=ctx, L=L)
        build_B(ctx=ctx, L=L, last=(L == 1))
    ctx.P.emit()
    return ctx.nc


def host_fused_inputs(inp):
    cm, mask, perm, ident = _consts_np()
    per_layer = []
    for i in range(2):
        mA = host_A_inputs(i, inp["x"], inp["ctx"], inp)
        per_layer.append(mA)
    maps = []
    for r in range(8):
        b, seg = r // 4, r % 4
        flags = np.zeros((128, 16), np.float32)
        for s2 in range(4):
            flags[:, s2] = 1.0 if s2 < seg else 0.0
            flags[:, 4 + s2] = 1.0 if s2 > seg else 0.0
            flags[:, 8 + s2] = 1.0 if s2 == seg - 1 else 0.0
            flags[:, 12 + s2] = 1.0 if s2 == seg + 1 else 0.0
        smask = np.stack([mask[2], mask[3], mask[2] if seg > 0 else 0 * mask[2], mask[3] if seg < 3 else 0 * mask[3]], axis=0)
        m0 = per_layer[0][r]
        m = {"x_all": m0["x_all"], "cvec": m0["cvec"], "cmats": cm, "masks": mask, "perm": perm, "ident": ident,
             "ropec": m0["ropec"], "ropes": m0["ropes"], "flags16": flags,
             "smask": np.ascontiguousarray(smask.astype(np.float32)),
             "w_router": inp["w_router"], "b_router": inp["b_router"], "fnorm": inp["final_norm"]}
        for i in range(2):
            mi = per_layer[i][r]
            for k in A_LAYER:
                m[f"{k}_{i}"] = mi[k]
            m[f"w_out_{i}"] = inp["w_out"][i]
            m[f"norm2_{i}"] = inp["norm2"][i]
            m[f"gnorm_{i}"] = inp["gla_norm"][i]
            m[f"sink_{i}"] = inp["swa_sink"][i]
            m[f"w_gate_{i}"] = inp["w_gate"][i]
            m[f"w_up_{i}"] = inp["w_up"][i]
            m[f"w_down_{i}"] = inp["w_down"][i]
        maps.append(m)
    return maps


def kernel(**inputs):
    inp = {k: np.ascontiguousarray(np.asarray(v, dtype=np.float32)) for k, v in inputs.items()}
    nc = _get("F", build_fused)
    res = run_bass_kernel_spmd(nc, host_fused_inputs(inp), core_ids=list(range(8))).results
    y = np.stack([np.asarray(r["y_fin"]) for r in res], axis=0).reshape(2, 4 * TL, D)
    return np.ascontiguousarray(y.astype(np.float32))
```

```python
import contextlib
import numpy as np
import ml_dtypes
import concourse.bass as bass
import concourse.mybir as mybir
from concourse.bass_utils import run_bass_kernel_spmd

F32 = mybir.dt.float32
BF16 = mybir.dt.bfloat16
ALU = mybir.AluOpType
AF = mybir.ActivationFunctionType
AX = mybir.AxisListType
NPBF = ml_dtypes.bfloat16

D = 1024
NT_L = 16
NT_C = 2
NT = NT_L + NT_C
TL = NT_L * 128
EPS = 1e-6
NEXP = 16
DE = 512
ARENA_WORDS = 39600


class Tok:
    __slots__ = ("w", "r", "name", "excl")

    def __init__(self, name="", excl=False):
        self.w = None
        self.r = []
        self.name = name
        self.excl = excl


class Op:
    __slots__ = ("eng", "fn", "deps", "sig", "chan", "has_dep", "seq", "inc")

    def __init__(self, eng, fn, deps, chan, inc=16):
        self.eng = eng
        self.fn = fn
        self.deps = deps
        self.sig = None
        self.chan = chan
        self.has_dep = False
        self.seq = 0
        self.inc = inc


class Prog:
    ENGS = ("pe", "act", "dve", "pool", "sp")

    def __init__(self, nc):
        self.nc = nc
        self.ops = {e: [] for e in self.ENGS}
        self.stack = contextlib.ExitStack()
        self.nt = 0
        self.banks = []
        self.bank_i = 0
        self.nchan = 0
        self.nseq = 0
        self.cont = False
        self.arena = None
        self.in_scope = False
        self.arena_off = 0
        self.arena_words = 0

    def sb(self, name, shape, dt):
        if self.in_scope:
            nel = 1
            for d_ in shape[1:]:
                nel *= d_
            esz = 2 if dt == BF16 else 4
            words = (nel * esz + 3) // 4
            off = self.arena_off
            assert off + words <= self.arena_words, (name, off, words, self.arena_words)
            self.arena_off += words
            v = self.arena[0:shape[0], off:off + words]
            if dt == BF16:
                v = v.bitcast(BF16)[:, 0:nel]
            elif dt != F32:
                v = v.bitcast(dt)
            if len(shape) > 2:
                names = " ".join(f"a{i}" for i in range(len(shape) - 1))
                kw = {f"a{i}": shape[i + 1] for i in range(len(shape) - 2)}
                v = v.rearrange(f"p ({names}) -> p {names}", **kw)
            return v
        return self.stack.enter_context(self.nc.sbuf_tensor("sb_" + name, list(shape), dt))

    def open_scope(self, words=None):
        if self.arena is None:
            self.arena_words = words
            self.arena = self.stack.enter_context(self.nc.sbuf_tensor("sb_arena", [128, words], F32))
        self.arena_off = 0
        self.in_scope = True

    def close_scope(self):
        self.barrier()
        self.in_scope = False

    def ps(self, name, shape, dt=F32):
        return self.stack.enter_context(self.nc.psum_tensor("ps_" + name, list(shape), dt))

    def tok(self, name="", excl=False):
        self.nt += 1
        return Tok(name or f"t{self.nt}", excl)

    def newchan(self, name="c"):
        self.nchan += 1
        return f"{name}{self.nchan}"

    def make_banks(self, n):
        for i in range(n):
            self.banks.append((self.ps(f"bank{i}", [128, 512], F32), self.tok(f"bank{i}", True)))

    def bank(self):
        b = self.banks[self.bank_i % len(self.banks)]
        self.bank_i += 1
        return b

    def op(self, eng, fn, reads=(), writes=(), chan=None, inc=16):
        deps = []
        ex = [t for t in reads if t.excl]
        if ex:
            reads = [t for t in reads if not t.excl]
            writes = list(writes) + ex
        for t in reads:
            if t.w is not None:
                deps.append(t.w)
        for t in writes:
            if t.w is not None:
                deps.append(t.w)
            deps.extend(t.r)
        if eng == "pe" and self.cont:
            deps = [d for d in deps if d.eng != "pe"]
        self.cont = False
        o = Op(eng, fn, deps, chan, inc)
        self.nseq += 1
        o.seq = self.nseq
        for d in deps:
            d.has_dep = True
        for t in reads:
            t.r.append(o)
        for t in writes:
            t.w = o
            t.r = []
        self.ops[eng].append(o)
        return o

    def barrier(self):
        last = []
        for e in self.ENGS:
            for o in reversed(self.ops[e]):
                if o.chan is None and o.fn is not None:
                    last.append(o)
                    break
        seen = set()
        for e in self.ENGS:
            for o in reversed(self.ops[e]):
                if o.chan is not None and o.chan not in seen:
                    seen.add(o.chan)
                    last.append(o)
        for e in self.ENGS:
            o = Op(e, None, list(last), None)
            self.nseq += 1
            o.seq = self.nseq
            self.ops[e].append(o)
        for d in last:
            d.has_dep = True

    def dma(self, eng, out, in_, reads=(), writes=(), chan=None, **kw):
        assert chan is not None
        return self.op(eng, lambda e: e.dma_start(out=out, in_=in_, **kw), reads, writes, chan=chan)

    def emit(self, final_wait_eng="sp"):
        nc = self.nc
        sems = {}
        for e in self.ENGS:
            sems[e] = self.stack.enter_context(nc.semaphore(f"s_{e}"))
        cnt = {e: 0 for e in self.ENGS}
        ccnt = {}
        for e in ("pe", "act", "dve", "pool"):
            for o in reversed(self.ops[e]):
                if o.chan is None and o.fn is not None:
                    o.has_dep = True
                    break
        allops = sorted((o for e in self.ENGS for o in self.ops[e]), key=lambda o: o.seq)
        for o in allops:
            e = o.eng
            if o.chan is not None:
                if o.chan not in sems:
                    sems[o.chan] = self.stack.enter_context(nc.semaphore(f"c_{o.chan}"))
                    ccnt[o.chan] = 0
                ccnt[o.chan] += o.inc
                o.sig = (o.chan, ccnt[o.chan], o.inc)
            elif o.has_dep and o.fn is not None:
                cnt[e] += 1
                o.sig = (e, cnt[e], 1)
        final = dict(ccnt)
        for e in ("pe", "act", "dve", "pool"):
            if cnt[e]:
                final[e] = cnt[e]

        def run(e, handle, extra_final):
            waited = {}
            for o in self.ops[e]:
                need = {}
                for d in o.deps:
                    k, v, _ = d.sig
                    if need.get(k, 0) < v:
                        need[k] = v
                for k, v in need.items():
                    if waited.get(k, 0) < v:
                        handle.wait_ge(sems[k], v)
                        waited[k] = v
                if o.fn is None:
                    continue
                ins = o.fn(handle)
                if o.sig is not None:
                    ins.then_inc(sems[o.sig[0]], o.sig[2])
            if extra_final:
                for k, v in final.items():
                    if waited.get(k, 0) < v:
                        handle.wait_ge(sems[k], v)

        with nc.Block() as block:
            @block.tensor
            def _(h):
                run("pe", h, False)

            @block.scalar
            def _(h):
                run("act", h, False)

            @block.vector
            def _(h):
                run("dve", h, False)

            @block.gpsimd
            def _(h):
                run("pool", h, False)

            @block.sync
            def _(h):
                run("sp", h, True)
        self.stack.close()


class Rot:
    def __init__(self, P, name, shape, dt, n=2):
        self.bufs = [P.sb(f"{name}{i}", shape, dt) for i in range(n)]
        self.toks = [P.tok(f"{name}{i}") for i in range(n)]
        self.chans = [P.newchan(name) for i in range(n)]
        self.i = 0

    def next(self):
        j = self.i % len(self.bufs)
        self.i += 1
        return self.bufs[j], self.toks[j], self.chans[j]


def _consts_np():
    s = np.arange(128)[:, None]
    c = np.arange(128)[None, :]
    v = np.float32(-1.0 / 16.0)
    cm = np.zeros((4, 128, 128), np.float32)
    cm[0] = np.where(s <= c, v, 0)
    cm[1] = np.where(s >= c, v, 0)
    cm[2] = np.where(s > c, v, 0)
    cm[3] = np.where(s < c, v, 0)
    mask = np.zeros((4, 128, 128), np.float32)
    mask[0] = (s <= c)
    mask[1] = (s >= c)
    mask[2] = (s >= c)
    mask[3] = (s <= c)
    perm = np.zeros((128, 128), np.float32)
    for m in range(128):
        partner = m + 16 if (m % 32) < 16 else m - 16
        perm[partner, m] = 1.0
    ident = np.eye(128, dtype=np.float32)
    return cm, mask, perm, ident


def _rope_tables(pos0, n):
    t = pos0 + np.arange(n)
    row = (t // 64).astype(np.float32)
    col = (t % 64).astype(np.float32)
    inv = (np.float32(10000.0) ** (-(np.arange(16, dtype=np.float32) * np.float32(2.0) / np.float32(32.0)))).astype(np.float32)
    cos = np.zeros((128, n), np.float32)
    sin = np.zeros((128, n), np.float32)
    for p in range(128):
        d = p % 64
        f = d % 16
        pos = row if d < 32 else col
        ang = (pos * inv[f]).astype(np.float32)
        sgn = -1.0 if (d % 32) < 16 else 1.0
        cos[p] = np.cos(ang)
        sin[p] = sgn * np.sin(ang)
    return cos, sin


FUSED_ARENA = 52400
A_LAYER = ("w_ada", "b_ada", "norm1", "w_fm", "w_tm", "up_aug")
B_LAYER = ("w_out", "norm2", "gnorm", "sink", "w_gate", "w_up", "w_down")
B_FROM_A = {"modr": "o_mod", "qs": "o_qs", "ks": "o_ks", "vs": "o_vs", "g": "o_g", "of": "o_of", "ob": "o_ob",
            "qbf": "o_qbf", "qbb": "o_qbb", "st0": "o_st"}


class Ctx:
    def __init__(self):
        self.nc = bass.Bass("TRN2", target_bir_lowering=False)
        self.P = Prog(self.nc)
        self.P.make_banks(6)
        self.ptr = [self.P.ps(f"ptr{i}", [128, 8, 128], BF16) for i in range(2)]
        self.ptr_t = [self.P.tok(excl=True) for i in range(2)]
        self.P.open_scope(FUSED_ARENA)
        self.P.in_scope = False
        self.d = {}

    def ext_in(self, name, shape, dt=F32):
        if name not in self.d:
            self.d[name] = self.nc.dram_tensor(name, list(shape), dt, kind="ExternalInput").ap()
        return self.d[name]

    def internal(self, name, shape, dt=F32):
        if name not in self.d:
            self.d[name] = self.nc.dram_tensor(name, list(shape), dt).ap()
        return self.d[name]

    def inp(self, ph, L, name, shape, dt):
        if name == "x_all":
            return self.ext_in("x_all", shape, dt) if L == 0 else self.d["B0_x_out"]
        if ph == "A" and name in A_LAYER:
            return self.ext_in(f"{name}_{L}", shape, dt)
        if ph == "B" and name in B_LAYER:
            return self.ext_in(f"{name}_{L}", shape, dt)
        if ph == "B" and name in B_FROM_A:
            return self.d[f"A{L}_{B_FROM_A[name]}"]
        return self.ext_in(name, shape, dt)


def build_A(dbg_tiles=None, dbg_scan=True, dbg_lvl=99, dbg_stage=99, ctx=None, L=0):
    fused = ctx is not None
    nc = ctx.nc if fused else bass.Bass("TRN2", target_bir_lowering=False)

    def din(name, shape, dt=F32):
        if fused:
            return ctx.inp("A", L, name, shape, dt)
        return nc.dram_tensor(name, list(shape), dt, kind="ExternalInput").ap()

    def dout(name, shape, dt=F32):
        if fused:
            return ctx.internal(f"A{L}_{name}", shape, dt)
        return nc.dram_tensor(name, list(shape), dt, kind="ExternalOutput").ap()

    x_all = din("x_all", [NT * 128, D])
    cvec = din("cvec", [2, D])
    w_ada = din("w_ada", [D, 6 * D])
    b_ada = din("b_ada", [6 * D])
    norm1 = din("norm1", [D])
    w_fm = din("w_fm", [D, 1280])
    w_tm = din("w_tm", [D, 1440])
    up_aug = din("up_aug", [33, 512])
    cmats = din("cmats", [4, 128, 128])
    masks = din("masks", [4, 128, 128])
    perm = din("perm", [128, 128])
    identd = din("ident", [128, 128])
    ropec = din("ropec", [128, TL])
    ropes = din("ropes", [128, TL])

    o_mod = dout("o_mod", [2, 6 * D])
    o_qs = dout("o_qs", [128, 4, NT * 128], BF16)
    o_ks = dout("o_ks", [128, 2, NT * 128], BF16)
    o_vs = dout("o_vs", [NT * 128, 2, 65], BF16)
    o_g = dout("o_g", [NT * 128, 512], BF16)
    o_of = dout("o_of", [NT * 128, 512])
    o_ob = dout("o_ob", [NT * 128, 512])
    o_qbf = dout("o_qbf", [128, 2, TL], BF16)
    o_qbb = dout("o_qbb", [128, 2, TL], BF16)
    o_st = dout("o_st", [4, 128, 2, 256])
    o_pd = dout("o_pd", [128, 4])

    if fused:
        P, ptr, ptr_t = ctx.P, ctx.ptr, ctx.ptr_t
        P.nchan = 0
        P.open_scope()
        exp_all = ctx.internal(f"exp{L}", [128, 1414], F32)
        exp_f = exp_all[:, 0:1028]
        exp_b = exp_all.bitcast(BF16)[:, 2056:2828]
    else:
        P = Prog(nc)
        P.make_banks(6)
        ptr = [P.ps(f"ptr{i}", [128, 8, 128], BF16) for i in range(2)]
        ptr_t = [P.tok(excl=True) for i in range(2)]

    cm_sb = P.sb("cm_sb", [128, 4, 128], BF16)
    mk_sb = P.sb("mk_sb", [128, 2, 128], F32)
    perm_sb = P.sb("perm_sb", [128, 128], BF16)
    id_f = P.sb("id_f", [128, 128], F32)
    id_b = P.sb("id_b", [128, 128], BF16)
    up_sb = P.sb("up_sb", [33, 512], F32)
    t_const = P.tok("const")
    t_idb = P.tok("idb")
    P.dma("pool", cm_sb[:], cmats.rearrange("m p c -> p m c"), writes=[t_idb], chan="constb")
    P.dma("sp", mk_sb[:], masks[0:2].rearrange("m p c -> p m c"), writes=[t_const], chan="const")
    P.dma("pool", perm_sb[:], perm, writes=[t_idb], chan="constb")
    P.dma("sp", id_f[:], identd, writes=[t_const], chan="const")
    P.dma("sp", up_sb[:], up_aug, writes=[t_const], chan="const")
    P.dma("pool", id_b[:], identd, writes=[t_idb], chan="constb")

    wfm_sb = P.sb("wfm_sb", [128, 8, 1280], BF16)
    wtm_sb = P.sb("wtm_sb", [128, 8, 1440], BF16)
    t_wfm = P.tok("wfm")
    t_wtm = P.tok("wtm")

    if dbg_lvl < 2:
        P.emit()
        return nc
    crow = P.sb("crow", [2, D], F32)
    cTa = P.sb("cTa", [128, 8, 2], F32)
    t_crow = P.tok("crow")
    t_cT = P.tok("cT")
    for k in range(8):
        P.dma("pool", wfm_sb[:, k, :], w_fm[k * 128:(k + 1) * 128, :], writes=[t_wfm], chan="wfm")
    for k in range(8):
        P.dma("pool", wtm_sb[:, k, :], w_tm[k * 128:(k + 1) * 128, :], writes=[t_wtm], chan="wtm")
    P.dma("sp", crow[:], cvec, writes=[t_crow], chan="crow")
    P.op("act", lambda e: e.activation(out=crow[:], in_=crow[:], func=AF.Silu), reads=[t_crow], writes=[t_crow])
    bkc, btc = P.bank()
    for k in range(8):
        P.cont = k > 0
        P.op("pe", lambda e, k=k: e.transpose(out=bkc[:, k * 2:k * 2 + 2], in_=crow[:, k * 128:(k + 1) * 128],
                                              identity=id_f[0:2, 0:2]),
             reads=[t_crow, t_const], writes=[btc])
    P.op("dve", lambda e: e.tensor_copy(out=cTa[:], in_=bkc[:, 0:16].rearrange("p (k j) -> p k j", j=2)),
         reads=[btc], writes=[t_cT])
    mod_sb = P.sb("mod_sb", [2, 2 * D], F32)
    n1_sb = P.sb("n1_sb", [2, D], F32)
    t_mod = P.tok("mod")
    t_n1 = P.tok("n1")
    P.dma("sp", n1_sb[:], norm1.unsqueeze(0).to_broadcast([2, D]), writes=[t_n1], chan="n1")
    wada = Rot(P, "wada", [128, 8, 256], F32, 1)
    bada = Rot(P, "bada", [2, 256], F32, 2)
    mst = Rot(P, "mst", [2, 256], F32, 2)

    mod_pending = {}

    def mod_load(sbk):
        c0 = sbk * 256
        wb, wt, wc = wada.next()
        P.dma("sp", wb[:], w_ada[:, c0:c0 + 256].rearrange("(k p) n -> p k n", p=128), writes=[wt], chan=wc)
        bb_, bbt, bbc = bada.next()
        P.dma("sp", bb_[:], b_ada[c0:c0 + 256].unsqueeze(0).to_broadcast([2, 256]), writes=[bbt], chan=bbc)
        mod_pending[sbk] = (wb, wt, bb_, bbt)

    def mod_compute(sbk):
        c0 = sbk * 256
        wb, wt, bb_, bbt = mod_pending.pop(sbk)
        bk, bt = P.bank()
        for k in range(8):
            P.cont = k > 0
            P.op("pe", lambda e, k=k: e.matmul(out=bk[0:2, 0:256], lhsT=cTa[:, k, :], rhs=wb[:, k, :],
                                               start=(k == 0), stop=(k == 7)),
                 reads=[t_cT, wt], writes=[bt])
        if sbk < 8:
            P.op("dve", lambda e: e.tensor_tensor(out=mod_sb[:, c0:c0 + 256], in0=bk[0:2, 0:256], in1=bb_[:], op=ALU.add),
                 reads=[bt, bbt], writes=[t_mod])
        else:
            mb_, mbt, mbc = mst.next()
            P.op("dve", lambda e: e.tensor_tensor(out=mb_[:], in0=bk[0:2, 0:256], in1=bb_[:], op=ALU.add),
                 reads=[bt, bbt], writes=[mbt])
            P.dma("sp", o_mod[:, c0:c0 + 256], mb_[:], reads=[mbt], chan=mbc)

    for sbk in range(8):
        mod_load(sbk)
        mod_compute(sbk)
    if dbg_lvl < 4:
        P.emit()
        return nc
    mch = P.newchan("omod")
    P.dma("sp", o_mod[:, 0:2 * D], mod_sb[:], reads=[t_mod], chan=mch)
    arow = P.sb("arow", [2, D], F32)
    t_arow = P.tok("arow")
    P.op("dve", lambda e: e.scalar_tensor_tensor(out=arow[:], in0=mod_sb[:, D:2 * D], scalar=1.0, in1=n1_sb[:],
                                                 op0=ALU.add, op1=ALU.mult),
         reads=[t_mod, t_n1], writes=[t_arow])
    if dbg_lvl < 5:
        P.emit()
        return nc
    sel = P.sb("sel", [2, 2, 128], F32)
    t_sel = P.tok("sel")
    P.op("pool", lambda e: e.memset(sel[:], 0.0), writes=[t_sel])
    P.op("pool", lambda e: e.affine_select(out=sel[:], in_=sel[:], pattern=[[-1, 2], [0, 128]],
                                           compare_op=ALU.not_equal, fill=1.0, base=0, channel_multiplier=1),
         reads=[t_sel], writes=[t_sel])
    if dbg_lvl < 6:
        P.emit()
        return nc
    AB = P.sb("AB", [128, 2, D], F32)
    t_AB = P.tok("AB")

    def build_AB(j):
        for which in range(2):
            for hb in range(2):
                bk, bt = P.bank()
                src = arow if which == 0 else mod_sb
                off = hb * 512
                P.op("pe", lambda e, src=src, off=off, bk=bk: e.matmul(out=bk[:], lhsT=sel[:, j, :],
                                                                        rhs=src[:, off:off + 512], start=True, stop=True),
                     reads=[t_sel, t_arow, t_mod], writes=[bt])
                P.op("act", lambda e, which=which, hb=hb, bk=bk: e.copy(out=AB[:, which, hb * 512:(hb + 1) * 512], in_=bk[:]),
                     reads=[bt], writes=[t_AB])

    xt = Rot(P, "xt", [128, D], F32, 2)
    h1 = P.sb("h1", [128, D], F32)
    t_h1 = P.tok("h1")
    hb_ = P.sb("hb", [128, D], BF16)
    t_hb = P.tok("hb")
    hT = Rot(P, "hT", [128, 8, 128], BF16, 2)
    ssq = P.sb("ssq", [128, 2], F32)
    t_ssq = P.tok("ssq")
    zs = P.sb("zs", [128, 32], F32)
    t_zs = P.tok("zs")
    zT = P.sb("zT", [33, 128], F32)
    t_zT = P.tok("zT")
    P.op("pool", lambda e: e.memset(zT[:], 1.0), writes=[t_zT])
    sp_ = P.sb("sp", [128, 512], BF16)
    t_sp = P.tok("sp")
    E = P.sb("E", [128, 4, 128], F32)
    Ei = P.sb("Ei", [128, 4, 128], F32)
    Ek = P.sb("Ek", [128, 512], F32)
    t_E, t_Ei, t_Ek = P.tok("E"), P.tok("Ei"), P.tok("Ek")
    qkg = P.sb("qkg", [128, 4, 128], F32)
    t_qkg = P.tok("qkg")
    ktok = P.sb("ktok", [128, 256], F32)
    t_ktok = P.tok("ktok")
    NF = 2
    qin = [[P.sb(f"qin0_{i}", [128, 2, 128], BF16) for i in range(NF)], [P.sb(f"qin1_{t}", [128, 2, 128], BF16) for t in range(NT)]]
    kin = [[P.sb(f"kin0_{i}", [128, 2, 128], BF16) for i in range(NF)], [P.sb(f"kin1_{t}", [128, 2, 128], BF16) for t in range(NT)]]
    kout = [[P.sb(f"kout0_{i}", [128, 256], BF16) for i in range(NF)], [P.sb(f"kout1_{t}", [128, 256], BF16) for t in range(NT)]]
    t_gl = [[P.tok(f"gl0_{i}") for i in range(NF)], [P.tok(f"gl1_{t}") for t in range(NT)]]
    vb = [P.sb(f"vb{t}", [128, 512], BF16) for t in range(NT)]
    t_vb = [P.tok(f"vb{t}") for t in range(NT)]
    decs = P.sb("decs", [128, NT, 4], F32)
    t_dec = [P.tok(f"dec{t}") for t in range(NT)]

    def gi(d, t):
        return (t % NF) if d == 0 else t

    gs = Rot(P, "gs", [128, 512], BF16, 2)
    qk = P.sb("qk", [128, 6, 128], BF16)
    t_qk = P.tok("qk")
    r1 = P.sb("r1", [128, 6, 128], F32)
    t_r1 = P.tok("r1")
    r2 = P.sb("r2", [128, 6, 128], F32)
    t_r2 = P.tok("r2")
    qko = Rot(P, "qko", [128, 6, 128], BF16, 2)
    vso = Rot(P, "vso", [128, 2, 65], BF16, 2)
    for b_, t_ in zip(vso.bufs, vso.toks):
        P.op("pool", lambda e, b_=b_: e.memset(b_[:], 1.0), writes=[t_])
    rcs = Rot(P, "rcs", [128, 2, 128], F32, 2)

    S = [P.sb(f"S{d}", [128, 2, 256], F32) for d in range(2)]
    Sb = [P.sb(f"Sb{d}", [128, 2, 256], BF16) for d in range(2)]
    cum = [P.sb(f"cum{d}", [128, 2], F32) for d in range(2)]
    t_S = [P.tok(f"S{d}") for d in range(2)]
    t_Sb = [P.tok(f"Sb{d}") for d in range(2)]
    t_cum = [P.tok(f"cum{d}") for d in range(2)]
    ATs = P.sb("ATs", [128, 4, 128], BF16)
    t_ATs = P.tok("ATs")
    ost = Rot(P, "ost", [128, 512], F32, 2)
    qbst = Rot(P, "qbst", [128, 2, 128], BF16, 2)
    stch = P.newchan("st")

    def reset_state(d):
        P.op("pool", lambda e: e.memset(S[d][:], 0.0), writes=[t_S[d]])
        P.op("pool", lambda e: e.memset(Sb[d][:], 0.0), writes=[t_Sb[d]])
        P.op("pool", lambda e: e.memset(cum[d][:], 1.0), writes=[t_cum[d]])

    def scan_step(d, t):
        g = gi(d, t)
        tg_ = t_gl[d][g]
        q_, k_, ko_ = qin[d][g], kin[d][g], kout[d][g]
        bk, bt = P.bank()
        bu, but = P.bank()

        def mm_at(h):
            hh, pr = h % 2, h // 2
            P.op("pe", lambda e: e.matmul(
                out=bk[:, h * 128:(h + 1) * 128], lhsT=k_[hh * 64:(hh + 1) * 64, pr, :],
                rhs=q_[hh * 64:(hh + 1) * 64, pr, :], start=True, stop=True),
                 reads=[tg_], writes=[bt])

        def mm_u(pr):
            P.op("pe", lambda e: e.matmul(out=bu[:, pr * 256:(pr + 1) * 256],
                                          lhsT=ko_[:, pr * 128:(pr + 1) * 128],
                                          rhs=vb[t][:, pr * 256:(pr + 1) * 256], start=True, stop=True),
                 reads=[tg_, t_vb[t]], writes=[but])
        mm_at(0)
        mm_u(0)
        mm_at(1)
        mm_u(1)
        mm_at(2)
        mm_at(3)
        P.op("dve", lambda e: e.tensor_tensor(out=ATs[:], in0=bk[:].rearrange("p (h c) -> p h c", h=4),
                                              in1=mk_sb[:, d, :].unsqueeze(1).to_broadcast([128, 4, 128]), op=ALU.mult),
             reads=[bt, t_const], writes=[t_ATs])
        bo, bot = P.bank()
        for h in range(4):
            hh, pr = h % 2, h // 2
            P.op("pe", lambda e, h=h: e.matmul(out=bo[:, h * 128:(h + 1) * 128], lhsT=ATs[:, h, :],
                                               rhs=vb[t][:, h * 128:(h + 1) * 128], start=True, stop=False),
                 reads=[t_ATs, t_vb[t]], writes=[bot])
            P.op("pe", lambda e, h=h, hh=hh, pr=pr: e.matmul(
                out=bo[:, h * 128:(h + 1) * 128], lhsT=q_[hh * 64:(hh + 1) * 64, pr, :],
                rhs=Sb[d][hh * 64:(hh + 1) * 64, pr, hh * 128:(hh + 1) * 128], start=False, stop=True),
                 reads=[tg_, t_Sb[d]], writes=[bot])
        ob, ot, oc = ost.next()
        P.op("act", lambda e: e.copy(out=ob[:], in_=bo[:]), reads=[bot], writes=[ot])
        dst = o_of if d == 0 else o_ob
        P.dma("sp", dst[t * 128:(t + 1) * 128, :], ob[:], reads=[ot], chan=oc)
        for pr in range(2):
            P.op("dve", lambda e, pr=pr: e.scalar_tensor_tensor(
                out=S[d][:, pr, :], in0=S[d][:, pr, :], scalar=decs[:, t, d * 2 + pr:d * 2 + pr + 1],
                in1=bu[:, pr * 256:(pr + 1) * 256], op0=ALU.mult, op1=ALU.add),
                 reads=[but, t_S[d], t_dec[t]], writes=[t_S[d]])
        P.op("act", lambda e: e.copy(out=Sb[d][:], in_=S[d][:]), reads=[t_S[d]], writes=[t_Sb[d]])
        if t < NT_L:
            qb, qt, qc = qbst.next()
            for pr in range(2):
                P.op("dve", lambda e, pr=pr: e.tensor_scalar(
                    out=qb[:, pr, :], in0=q_[:, pr, :], scalar1=cum[d][:, pr:pr + 1], scalar2=None, op0=ALU.mult),
                     reads=[tg_, t_cum[d]], writes=[qt])
            dstq = o_qbf if d == 0 else o_qbb
            P.dma("sp", dstq[:, :, t * 128:(t + 1) * 128], qb[:], reads=[qt], chan=qc)
            P.op("dve", lambda e: e.tensor_tensor(out=cum[d][:], in0=cum[d][:], in1=decs[:, t, d * 2:d * 2 + 2], op=ALU.mult),
                 reads=[t_cum[d], t_dec[t]], writes=[t_cum[d]])

    def export_state(d, slot):
        P.dma("sp", o_st[slot], S[d][:], reads=[t_S[d]], chan=stch)
        if fused and slot >= 2:
            P.dma("sp", exp_f[:, (slot - 2) * 512:(slot - 1) * 512].rearrange("p (a c) -> p a c", a=2), S[d][:],
                  reads=[t_S[d]], chan=stch)
            P.dma("sp", exp_f[:, 1024 + d * 2:1026 + d * 2], cum[d][:], reads=[t_cum[d]], chan=stch)

    xload = {}

    def load_x(t):
        xb, xtok, xc = xt.next()
        P.dma("sp", xb[:], x_all[t * 128:(t + 1) * 128, :], writes=[xtok], chan=xc)
        rb, rt, rch = None, None, None
        if t < NT_L:
            rb, rt, rch = rcs.next()
            P.dma("sp", rb[:, 0, :], ropec[:, t * 128:(t + 1) * 128], writes=[rt], chan=rch)
            P.dma("sp", rb[:, 1, :], ropes[:, t * 128:(t + 1) * 128], writes=[rt], chan=rch)
        xload[t] = (xb, xtok, rb, rt)

    def prep_tile(t, t_next):
        is_ctx = t >= NT_L
        xb, xtok, rb, rt = xload.pop(t)
        if t_next is not None:
            load_x(t_next)
        P.op("act", lambda e: e.activation(out=hb_[:], in_=xb[:], func=AF.Square, accum_out=ssq[:, 0:1]),
             reads=[xtok], writes=[t_ssq, t_hb])
        P.op("act", lambda e: e.activation(out=ssq[:, 1:2], in_=ssq[:, 0:1], func=AF.Ln, scale=1.0 / D, bias=EPS),
             reads=[t_ssq], writes=[t_ssq])
        P.op("act", lambda e: e.activation(out=ssq[:, 1:2], in_=ssq[:, 1:2], func=AF.Exp, scale=-0.5),
             reads=[t_ssq], writes=[t_ssq])
        P.op("dve", lambda e: e.scalar_tensor_tensor(out=h1[:], in0=xb[:], scalar=ssq[:, 1:2], in1=AB[:, 0, :],
                                                     op0=ALU.mult, op1=ALU.mult),
             reads=[xtok, t_ssq, t_AB], writes=[t_h1])
        P.op("pool", lambda e: e.tensor_tensor(out=hb_[:], in0=h1[:], in1=AB[:, 1, :], op=ALU.add),
             reads=[t_h1, t_AB], writes=[t_hb])
        if dbg_stage <= 1:
            return
        pi = t % 2
        for k in range(8):
            P.cont = k > 0
            P.op("pe", lambda e, k=k: e.transpose(out=ptr[pi][:, k, :], in_=hb_[:, k * 128:(k + 1) * 128], identity=id_b[:]),
                 reads=[t_hb, t_idb], writes=[ptr_t[pi]])
        hTb, hTt, _ = hT.next()
        P.op("act", lambda e: e.copy(out=hTb[:], in_=ptr[pi][:]), reads=[ptr_t[pi]], writes=[hTt])
        if dbg_stage <= 2:
            return
        bA, tA = P.bank()
        bB, tB = P.bank()
        bC, tC = P.bank()
        for cc in (0, 4, 8, 1, 5, 9, 2, 6, 3, 7):
            if cc < 4:
                dst_, tk = bA[:, cc * 128:(cc + 1) * 128], tA
            elif cc < 8:
                dst_, tk = bB[:, (cc - 4) * 128:(cc - 3) * 128], tB
            else:
                dst_, tk = bC[:, (cc - 8) * 128:(cc - 7) * 128], tC
            for k in range(8):
                P.cont = k > 0
                P.op("pe", lambda e, k=k, cc=cc, dst_=dst_: e.matmul(out=dst_, lhsT=wfm_sb[:, k, cc * 128:(cc + 1) * 128],
                                                                      rhs=hTb[:, k, :], start=(k == 0), stop=(k == 7)),
                     reads=[t_wfm, hTt], writes=[tk])
        bV, tV = P.bank()
        bG, tG = P.bank()
        bK, tK = P.bank()
        for (bk, tk, c0, cw) in ((bV, tV, 0, 512), (bG, tG, 512, 512), (bK, tK, 1024, 416)):
            for k in range(8):
                P.cont = k > 0
                P.op("pe", lambda e, k=k, bk=bk, c0=c0, cw=cw: e.matmul(out=bk[:, 0:cw], lhsT=hTb[:, k, :],
                                                                        rhs=wtm_sb[:, k, c0:c0 + cw],
                                                                        start=(k == 0), stop=(k == 7)),
                     reads=[t_wtm, hTt], writes=[tk])
        if dbg_stage <= 3:
            return
        P.op("dve", lambda e: e.tensor_copy(out=qkg[:], in_=bA[:].rearrange("p (a c) -> p a c", a=4)), reads=[tA], writes=[t_qkg])
        if dbg_stage <= 3.1:
            return
        qb_, qt_, qc_ = qko.next()
        if is_ctx:
            P.op("act", lambda e: e.copy(out=qb_[:, 0:4, :], in_=bB[:].rearrange("p (a c) -> p a c", a=4)),
                 reads=[tB], writes=[qt_])
            P.op("act", lambda e: e.copy(out=qb_[:, 4:6, :], in_=bC[:, 0:256].rearrange("p (a c) -> p a c", a=2)),
                 reads=[tC], writes=[qt_])
        else:
            P.op("act", lambda e: e.copy(out=qk[:, 0:4, :], in_=bB[:].rearrange("p (a c) -> p a c", a=4)),
                 reads=[tB], writes=[t_qk])
            P.op("act", lambda e: e.copy(out=qk[:, 4:6, :], in_=bC[:, 0:256].rearrange("p (a c) -> p a c", a=2)),
                 reads=[tC], writes=[t_qk])
        if dbg_stage <= 3.2:
            return
        P.op("act", lambda e: e.copy(out=vb[t][:], in_=bV[:]), reads=[tV], writes=[t_vb[t]])
        if dbg_stage <= 3.3:
            return
        gb, gt, gc = gs.next()
        P.op("dve", lambda e: e.tensor_copy(out=gb[:], in_=bG[:]), reads=[tG], writes=[gt])
        P.dma("sp", o_g[t * 128:(t + 1) * 128, :], gb[:], reads=[gt], chan=gc)
        if dbg_stage <= 3.4:
            return
        vsb, vst, vsc = vso.next()
        P.op("act", lambda e: e.copy(out=vsb[:, :, 0:64], in_=bK[:, 288:416].rearrange("p (j d) -> p j d", j=2)),
             reads=[tK], writes=[vst])
        P.dma("sp", o_vs[t * 128:(t + 1) * 128, :, :], vsb[:], reads=[vst], chan=vsc)
        if dbg_stage <= 3.5:
            return
        P.op("act", lambda e: e.copy(out=zs[:], in_=bK[:, 256:288]), reads=[tK], writes=[t_zs])
        if dbg_stage <= 3.6:
            return
        P.op("dve", lambda e: e.tensor_copy(out=ktok[:], in_=bK[:, 0:256]), reads=[tK], writes=[t_ktok])
        if dbg_stage <= 4:
            return
        bz, tz = P.bank()
        P.op("pe", lambda e: e.transpose(out=bz[0:32, 0:128], in_=zs[:], identity=id_f[:]),
             reads=[t_zs, t_const], writes=[tz])
        P.op("dve", lambda e: e.tensor_copy(out=zT[0:32, :], in_=bz[0:32, 0:128]), reads=[tz], writes=[t_zT])
        bp, tp = P.bank()
        P.op("pe", lambda e: e.matmul(out=bp[:], lhsT=zT[:], rhs=up_sb[:], start=True, stop=True),
             reads=[t_zT, t_const], writes=[tp])
        P.op("act", lambda e: e.activation(out=Ek[:], in_=bp[:], func=AF.Exp, scale=-1.0), reads=[tp], writes=[t_Ek])
        P.op("act", lambda e: e.activation(out=sp_[:], in_=Ek[:], func=AF.Ln, bias=1.0, scale=1.0),
             reads=[t_Ek], writes=[t_sp])
        if dbg_stage <= 5:
            return
        bb, tb = P.bank()
        bg, tg = P.bank()

        def mm_b(ci):
            P.op("pe", lambda e: e.matmul(out=bb[:, ci * 128:(ci + 1) * 128], lhsT=sp_[:, ci * 128:(ci + 1) * 128],
                                          rhs=cm_sb[:, ci // 2, :], start=True, stop=True),
                 reads=[t_sp, t_idb], writes=[tb])

        def mm_g(d):
            P.op("pe", lambda e: e.matmul(out=bg[:, d * 256:(d + 1) * 256], lhsT=cm_sb[:, 2 + d, :],
                                          rhs=sp_[:, d * 256:(d + 1) * 256], start=True, stop=True),
                 reads=[t_sp, t_idb], writes=[tg])
        mm_b(0)
        mm_g(0)
        mm_b(1)
        mm_g(1)
        mm_b(2)
        mm_b(3)
        P.op("act", lambda e: e.activation(out=E[:], in_=bb[:].rearrange("p (a c) -> p a c", a=4), func=AF.Exp),
             reads=[tb], writes=[t_E])
        P.op("act", lambda e: e.activation(out=Ei[:], in_=bb[:].rearrange("p (a c) -> p a c", a=4), func=AF.Exp, scale=-1.0),
             reads=[tb], writes=[t_Ei])
        P.op("act", lambda e: e.activation(out=Ek[:], in_=bg[:], func=AF.Exp), reads=[tg], writes=[t_Ek])
        if dbg_stage <= 6:
            return
        for d in range(2):
            g = gi(d, t)
            P.op("dve", lambda e, d=d, g=g: e.scalar_tensor_tensor(out=qin[d][g][:], in0=qkg[:, 0:2, :], scalar=0.125,
                                                                   in1=E[:, d * 2:d * 2 + 2, :], op0=ALU.mult, op1=ALU.mult),
                 reads=[t_qkg, t_E], writes=[t_gl[d][g]])
            P.op("pool", lambda e, d=d, g=g: e.tensor_tensor(out=kin[d][g][:], in0=qkg[:, 2:4, :], in1=Ei[:, d * 2:d * 2 + 2, :],
                                                             op=ALU.mult),
                 reads=[t_qkg, t_Ei], writes=[t_gl[d][g]])
            P.op("dve", lambda e, d=d, g=g: e.tensor_tensor(out=kout[d][g][:], in0=ktok[:], in1=Ek[:, d * 256:(d + 1) * 256],
                                                            op=ALU.mult),
                 reads=[t_ktok, t_Ek], writes=[t_gl[d][g]])
        P.op("act", lambda e: e.copy(out=decs[:, t, 0:2], in_=E[:, 0:2, 127]), reads=[t_E], writes=[t_dec[t]])
        P.op("act", lambda e: e.copy(out=decs[:, t, 2:4], in_=E[:, 2:4, 0]), reads=[t_E], writes=[t_dec[t]])
        if dbg_stage <= 7:
            return
        if not is_ctx:
            bp1, tp1 = P.bank()
            bp2, tp2 = P.bank()
            P.op("pe", lambda e: e.matmul(out=bp1[:], lhsT=perm_sb[:], rhs=qk[:, 0:4, :].rearrange("p a c -> p (a c)"),
                                          start=True, stop=True), reads=[t_qk, t_idb], writes=[tp1])
            P.op("pe", lambda e: e.matmul(out=bp2[:, 0:256], lhsT=perm_sb[:], rhs=qk[:, 4:6, :].rearrange("p a c -> p (a c)"),
                                          start=True, stop=True), reads=[t_qk, t_idb], writes=[tp2])
            cs = rb[:, 0, :].unsqueeze(1)
            sn = rb[:, 1, :].unsqueeze(1)
            P.op("dve", lambda e: e.tensor_tensor(out=r2[:, 0:4, :], in0=bp1[:].rearrange("p (a c) -> p a c", a=4),
                                                  in1=sn.to_broadcast([128, 4, 128]), op=ALU.mult),
                 reads=[tp1, rt], writes=[t_r2])
            P.op("dve", lambda e: e.tensor_tensor(out=r2[:, 4:6, :], in0=bp2[:, 0:256].rearrange("p (a c) -> p a c", a=2),
                                                  in1=sn.to_broadcast([128, 2, 128]), op=ALU.mult),
                 reads=[tp2, rt], writes=[t_r2])
            P.op("pool", lambda e: e.tensor_tensor(out=r1[:], in0=qk[:], in1=cs.to_broadcast([128, 6, 128]), op=ALU.mult),
                 reads=[t_qk, rt], writes=[t_r1])
            P.op("pool", lambda e: e.tensor_tensor(out=qb_[:], in0=r1[:], in1=r2[:], op=ALU.add),
                 reads=[t_r1, t_r2], writes=[qt_])
        P.dma("sp", o_qs[:, :, t * 128:(t + 1) * 128], qb_[:, 0:4, :], reads=[qt_], chan=qc_)
        P.dma("sp", o_ks[:, :, t * 128:(t + 1) * 128], qb_[:, 4:6, :], reads=[qt_], chan=qc_)
        if fused and t in (0, NT_L - 1):
            side = 0 if t == 0 else 1
            P.dma("sp", exp_b[:, side * 256:(side + 1) * 256].rearrange("p (j c) -> p j c", j=2), qb_[:, 4:6, :],
                  reads=[qt_], chan=qc_)
            P.dma("sp", exp_b[:, 512 + side * 130:512 + (side + 1) * 130].rearrange("p (j d) -> p j d", j=2), vsb[:],
                  reads=[vst], chan=vsc)

    reset_state(0)
    reset_state(1)
    order = [NT_L, NT_L + 1] + list(range(NT_L))
    full = dbg_tiles is None
    if not full:
        order = order[:dbg_tiles]
    if order:
        load_x(order[0])
        build_AB(1)
    for i, t in enumerate(order):
        if t == 0:
            build_AB(0)
        prep_tile(t, order[i + 1] if i + 1 < len(order) else None)
        if t == 0:
            export_state(0, 0)
            reset_state(0)
        if dbg_scan:
            scan_step(0, t)
    if full:
        export_state(0, 2)
        P.dma("sp", o_pd[:, 0:2], cum[0][:], reads=[t_cum[0]], chan=stch)
        mod_load(8)
        for t in [NT_L + 1, NT_L]:
            scan_step(1, t)
        export_state(1, 1)
        reset_state(1)
        for jj, t in enumerate(range(NT_L - 1, -1, -1)):
            mod_compute(8 + jj)
            if jj + 1 < 16:
                mod_load(8 + jj + 1)
            scan_step(1, t)
        export_state(1, 3)
        P.dma("sp", o_pd[:, 2:4], cum[1][:], reads=[t_cum[1]], chan=stch)
    if fused:
        P.close_scope()
        gat_all = ctx.internal(f"gat{L}", [512, 1414], F32)
        rg = [[0, 1, 2, 3], [4, 5, 6, 7]]
        P.op("pool", lambda e: e.collective_compute("AllGather", ALU.bypass, replica_groups=rg,
                                                    ins=[exp_all.opt()], outs=[gat_all.opt()]), chan="cc", inc=1)
        P.barrier()
        return None
    P.emit()
    return nc


def build_B(ctx=None, L=0, last=True):
    fused = ctx is not None
    nc = ctx.nc if fused else bass.Bass("TRN2", target_bir_lowering=False)

    def din(name, shape, dt=F32):
        if fused:
            return ctx.inp("B", L, name, shape, dt)
        return nc.dram_tensor(name, list(shape), dt, kind="ExternalInput").ap()

    def dout(name, shape, dt=F32):
        if fused:
            if name == "y_fin" and last:
                return nc.dram_tensor("y_fin", list(shape), dt, kind="ExternalOutput").ap()
            return ctx.internal(f"B{L}_{name}", shape, dt)
        return nc.dram_tensor(name, list(shape), dt, kind="ExternalOutput").ap()

    x_all = din("x_all", [NT * 128, D])
    modr = din("modr", [2, 6 * D])
    qs_d = din("qs", [128, 4, NT * 128], BF16)
    ks_d = din("ks", [128, 2, NT * 128], BF16)
    vs_d = din("vs", [NT * 128, 2, 65], BF16)
    if not fused:
        kh_d = din("kh", [128, 2, 2, 128], BF16)
        vh_d = din("vh", [2, 128, 2, 65], BF16)
    g_d = din("g", [NT * 128, 512], BF16)
    of_d = din("of", [NT * 128, 512])
    ob_d = din("ob", [NT * 128, 512])
    qbf_d = din("qbf", [128, 2, TL], BF16)
    qbb_d = din("qbb", [128, 2, TL], BF16)
    st0_d = din("st0", [2, 128, 2, 256])
    if fused:
        gat_f = ctx.d[f"gat{L}"].rearrange("(s p) w -> p s w", p=128)[:, :, 0:1028]
        gat_b = ctx.d[f"gat{L}"].bitcast(BF16).rearrange("(s p) w -> p s w", p=128)[:, :, 2056:2828]
        flg_d = din("flags16", [128, 16])
        NFL = 16
    else:
        stF_d = din("stF", [4, 128, 2, 256])
        stB_d = din("stB", [4, 128, 2, 256])
        pd_d = din("pd", [4, 128, 4])
        flg_d = din("flags", [128, 8])
        NFL = 8
    smask_d = din("smask", [4, 128, 128])
    identd = din("ident", [128, 128])
    w_out = din("w_out", [D, D])
    norm2 = din("norm2", [D])
    gnorm = din("gnorm", [512])
    sink = din("sink", [8])
    w_router = din("w_router", [D, NEXP])
    b_router = din("b_router", [NEXP])
    w_gate = din("w_gate", [NEXP, D, DE])
    w_up = din("w_up", [NEXP, D, DE])
    w_down = din("w_down", [NEXP, DE, D])
    fnorm = din("fnorm", [D])

    x_out = dout("x_out", [NT * 128, D])
    y_fin = dout("y_fin", [TL, D])

    if fused:
        P, ptr, ptr_t = ctx.P, ctx.ptr, ctx.ptr_t
        P.nchan = 0
        P.open_scope()
    else:
        P = Prog(nc)
        P.make_banks(6)
        ptr = [P.ps(f"ptr{i}", [128, 8, 128], BF16) for i in range(2)]
        ptr_t = [P.tok(excl=True) for i in range(2)]
        P.open_scope(FUSED_ARENA)

    h2T = P.sb("h2T", [128, 8, NT * 128], BF16)
    t_h2T = [P.tok(f"h2T{t}") for t in range(NT)]
    rw = P.sb("rw", [128, NT, NEXP], F32)
    t_rw = [P.tok(f"rw{t}") for t in range(NT)]
    G2 = P.sb("G2", [128, 2, D], F32)
    fn_sb = P.sb("fn_sb", [128, D], F32)
    id_f = P.sb("id_f", [128, 128], F32)
    id_b = P.sb("id_b", [128, 128], BF16)
    t_c2 = P.tok("c2")
    t_idb = P.tok("idb")
    for j in range(2):
        P.dma("sp", G2[:, j, :], modr[j, 5 * D:6 * D].unsqueeze(0).to_broadcast([128, D]), writes=[t_c2], chan="c2")
    P.dma("sp", fn_sb[:], fnorm.unsqueeze(0).to_broadcast([128, D]), writes=[t_c2], chan="c2")
    P.dma("sp", id_f[:], identd, writes=[t_c2], chan="c2")
    P.dma("pool", id_b[:], identd, writes=[t_idb], chan="c2b")

    persist_end = P.arena_off
    qs = P.sb("qs", [128, 4, NT * 128], BF16)
    ks = P.sb("ks", [128, 2, NT * 128], BF16)
    vs = P.sb("vs", [128, NT, 2, 65], BF16)
    kh = P.sb("kh", [128, 2, 2, 128], BF16)
    vh = P.sb("vh", [128, 2, 2, 65], BF16)
    qbf = P.sb("qbf", [128, 2, TL], BF16)
    qbb = P.sb("qbb", [128, 2, TL], BF16)
    smask = P.sb("smask", [128, 4, 128], BF16)
    wo = P.sb("wo", [128, 8, D], BF16)
    gn_sb = P.sb("gn_sb", [128, 512], F32)
    es_sb = P.sb("es_sb", [128, 8], F32)
    br_sb = P.sb("br_sb", [128, NEXP], F32)
    wr_sb = P.sb("wr_sb", [128, 8, NEXP], F32)
    n2_sb = P.sb("n2_sb", [128, D], F32)
    flg = P.sb("flg", [128, NFL], F32)
    t_in = P.tok("in")
    t_inb = P.tok("inb")
    P.dma("sp", qs[:], qs_d, writes=[t_in], chan="in")
    P.dma("sp", ks[:], ks_d, writes=[t_in], chan="in")
    P.dma("sp", vs[:], vs_d.rearrange("(t p) j d -> p t j d", p=128), writes=[t_in], chan="in")
    if not fused:
        P.dma("sp", kh[:], kh_d, writes=[t_in], chan="in")
        P.dma("sp", vh[:], vh_d.rearrange("s p j d -> p s j d"), writes=[t_in], chan="in")
    P.dma("sp", qbf[:], qbf_d, writes=[t_in], chan="in")
    P.dma("sp", qbb[:], qbb_d, writes=[t_in], chan="in")
    P.dma("sp", gn_sb[:], gnorm.unsqueeze(0).to_broadcast([128, 512]), writes=[t_in], chan="in")
    P.dma("sp", es_sb[:], sink.unsqueeze(0).to_broadcast([128, 8]), writes=[t_in], chan="in")
    P.dma("sp", br_sb[:], b_router.unsqueeze(0).to_broadcast([128, NEXP]), writes=[t_in], chan="in")
    P.dma("sp", wr_sb[:], w_router.rearrange("(k p) e -> p k e", p=128), writes=[t_in], chan="in")
    P.dma("sp", n2_sb[:], norm2.unsqueeze(0).to_broadcast([128, D]), writes=[t_in], chan="in")
    P.dma("sp", flg[:], flg_d, writes=[t_in], chan="in")
    P.dma("pool", smask[:], smask_d.rearrange("m p c -> p m c"), writes=[t_inb], chan="inb")
    for k in range(8):
        P.dma("pool", wo[:, k, :], w_out[k * 128:(k + 1) * 128, :], writes=[t_inb], chan="inb")
    if fused:
        ck = P.sb("ck", [128, 4, 512], BF16)
        cv = P.sb("cv", [128, 4, 260], BF16)
        P.dma("sp", ck[:], gat_b[:, :, 0:512], writes=[t_in], chan="in")
        P.dma("sp", cv[:], gat_b[:, :, 512:772], writes=[t_in], chan="in")
    P.op("act", lambda e: e.activation(out=es_sb[:], in_=es_sb[:], func=AF.Exp), reads=[t_in], writes=[t_in])
    if fused:
        for side in range(2):
            src_half = 1 - side
            for s2 in range(4):
                fcol = flg[:, 8 + side * 4 + s2:9 + side * 4 + s2]
                kin_ = ck[:, s2, src_half * 256:(src_half + 1) * 256].rearrange("p (j c) -> p j c", j=2)
                vin_ = cv[:, s2, src_half * 130:(src_half + 1) * 130].rearrange("p (j d) -> p j d", j=2)
                if s2 == 0:
                    P.op("dve", lambda e, side=side, fcol=fcol, kin_=kin_: e.tensor_scalar(
                        out=kh[:, :, side, :], in0=kin_, scalar1=fcol, scalar2=None, op0=ALU.mult), reads=[t_in], writes=[t_in])
                    P.op("dve", lambda e, side=side, fcol=fcol, vin_=vin_: e.tensor_scalar(
                        out=vh[:, side, :, :], in0=vin_, scalar1=fcol, scalar2=None, op0=ALU.mult), reads=[t_in], writes=[t_in])
                else:
                    P.op("dve", lambda e, side=side, fcol=fcol, kin_=kin_: e.scalar_tensor_tensor(
                        out=kh[:, :, side, :], in0=kin_, scalar=fcol, in1=kh[:, :, side, :], op0=ALU.mult, op1=ALU.add),
                         reads=[t_in], writes=[t_in])
                    P.op("dve", lambda e, side=side, fcol=fcol, vin_=vin_: e.scalar_tensor_tensor(
                        out=vh[:, side, :, :], in0=vin_, scalar=fcol, in1=vh[:, side, :, :], op0=ALU.mult, op1=ALU.add),
                         reads=[t_in], writes=[t_in])

    Sin = [P.sb(f"Sin{d}", [128, 2, 256], F32) for d in range(2)]
    Sinb = [P.sb(f"Sinb{d}", [128, 2, 256], BF16) for d in range(2)]
    t_Sin = [P.tok(f"Sin{d}") for d in range(2)]
    Fst = P.sb("Fst", [128, 2, 4, 2, 256], F32)
    pds = P.sb("pds", [128, 4, 4], F32)
    tmpS = P.sb("tmpS", [128, 256], F32)
    t_tmpS = P.tok("tmpS")
    t_F = P.tok("F")
    P.dma("sp", Sin[0][:], st0_d[0], writes=[t_Sin[0]], chan="st")
    P.dma("sp", Sin[1][:], st0_d[1], writes=[t_Sin[1]], chan="st")
    if fused:
        P.dma("sp", Fst[:, 0].rearrange("p s a c -> p s (a c)"), gat_f[:, :, 0:512], writes=[t_F], chan="st")
        P.dma("sp", Fst[:, 1].rearrange("p s a c -> p s (a c)"), gat_f[:, :, 512:1024], writes=[t_F], chan="st")
        P.dma("sp", pds[:], gat_f[:, :, 1024:1028], writes=[t_F], chan="st")
    else:
        P.dma("sp", Fst[:, 0], stF_d.rearrange("s p a c -> p s a c"), writes=[t_F], chan="st")
        P.dma("sp", Fst[:, 1], stB_d.rearrange("s p a c -> p s a c"), writes=[t_F], chan="st")
        P.dma("sp", pds[:], pd_d.rearrange("s p c -> p s c"), writes=[t_F], chan="st")
    for d in range(2):
        segs = range(4) if d == 0 else range(3, -1, -1)
        for sg in segs:
            for pr in range(2):
                P.op("dve", lambda e, d=d, sg=sg, pr=pr: e.scalar_tensor_tensor(
                    out=tmpS[:], in0=Sin[d][:, pr, :], scalar=pds[:, sg, d * 2 + pr:d * 2 + pr + 1], in1=Fst[:, d, sg, pr, :],
                    op0=ALU.mult, op1=ALU.add), reads=[t_Sin[d], t_F, t_in], writes=[t_tmpS])
                P.op("dve", lambda e, d=d, pr=pr: e.tensor_tensor(out=tmpS[:], in0=tmpS[:], in1=Sin[d][:, pr, :], op=ALU.subtract),
                     reads=[t_Sin[d], t_tmpS], writes=[t_tmpS])
                P.op("dve", lambda e, d=d, sg=sg, pr=pr: e.scalar_tensor_tensor(
                    out=Sin[d][:, pr, :], in0=tmpS[:], scalar=flg[:, d * 4 + sg:d * 4 + sg + 1], in1=Sin[d][:, pr, :],
                    op0=ALU.mult, op1=ALU.add), reads=[t_tmpS, t_in, t_Sin[d]], writes=[t_Sin[d]])
        P.op("act", lambda e, d=d: e.copy(out=Sinb[d][:], in_=Sin[d][:]), reads=[t_Sin[d]], writes=[t_Sin[d]])

    MT = P.sb("MT", [128, 3, D], F32)
    t_MT = P.tok("MT")
    mtch = P.newchan("mt")

    def build_MT(j):
        P.dma("sp", MT[:, 0, :], modr[j, 2 * D:3 * D].unsqueeze(0).to_broadcast([128, D]), writes=[t_MT], chan=mtch)
        P.dma("sp", MT[:, 1, :], modr[j, 4 * D:5 * D].unsqueeze(0).to_broadcast([128, D]), writes=[t_MT], chan=mtch)
        P.dma("sp", MT[:, 2, :], modr[j, 3 * D:4 * D].unsqueeze(0).to_broadcast([128, D]), writes=[t_MT], chan=mtch)
        P.op("dve", lambda e: e.scalar_tensor_tensor(out=MT[:, 1, :], in0=MT[:, 1, :], scalar=1.0, in1=n2_sb[:],
                                                     op0=ALU.add, op1=ALU.mult), reads=[t_MT, t_in], writes=[t_MT])

    xt = Rot(P, "xt", [128, D], F32, 1)
    oft = Rot(P, "oft", [128, 512], F32, 1)
    obt = Rot(P, "obt", [128, 512], F32, 1)
    gt_ = Rot(P, "gt", [128, 512], BF16, 2)
    PT = Rot(P, "PT", [128, 512], BF16, 6)
    mixR = Rot(P, "mix", [128, D], BF16, 2)
    mixbuf = {}

    def get_mix(t):
        if t not in mixbuf:
            mb, mt, _ = mixR.next()
            mixbuf[t] = (mb, mt)
        return mixbuf[t]
    mixT = P.sb("mixT", [128, 8, 128], BF16)
    t_mixT = P.tok("mixT")
    den = P.sb("den", [128, 8], F32)
    t_den = P.tok("den")
    ssq4 = P.sb("ssq4", [128, 8], F32)
    t_ssq4 = P.tok("ssq4")
    og = P.sb("og", [128, 512], F32)
    t_og = P.tok("og")
    sgl = P.sb("sgl", [128, 512], F32)
    t_sgl = P.tok("sgl")
    junk = P.sb("junk", [128, D], BF16)
    t_junk = P.tok("junk")
    x1 = Rot(P, "x1", [128, D], F32, 1)
    ssq = P.sb("ssq", [128, 2], F32)
    t_ssq = P.tok("ssq")
    h2 = P.sb("h2", [128, D], F32)
    t_h2 = P.tok("h2")
    h2Tf = P.sb("h2Tf", [128, 8, 128], F32)
    t_h2Tf = P.tok("h2Tf")
    rt_ = P.sb("rt", [128, 8, NEXP], F32)
    t_rt = P.tok("rt")

    def swa_tile(t):
        is_ctx = t >= NT_L
        mix, t_mix = get_mix(t)
        if is_ctx:
            keys = [("k", NT_L, None), ("k", NT_L + 1, None)]
        else:
            keys = []
            if t == 0:
                keys.append(("h", 0, 2))
            else:
                keys.append(("k", t - 1, 0))
            keys.append(("k", t, None))
            if t == NT_L - 1:
                keys.append(("h", 1, 3))
            else:
                keys.append(("k", t + 1, 1))
            keys += [("k", NT_L, None), ("k", NT_L + 1, None)]
        for j in range(2):
            pts = []
            for k0 in range(0, len(keys), 2):
                grp = keys[k0:k0 + 2]
                banks_ = [P.bank() for _ in grp]
                for g in range(4):
                    half, pair = g % 2, 2 * j + g // 2
                    for (kind, idx, mi), (bs, bst) in zip(grp, banks_):
                        if kind == "k":
                            kap = ks[half * 64:(half + 1) * 64, j, idx * 128:(idx + 1) * 128]
                        else:
                            kap = kh[half * 64:(half + 1) * 64, j, idx, :]
                        P.op("pe", lambda e, g=g, kap=kap, half=half, pair=pair, bs=bs: e.matmul(
                            out=bs[:, g * 128:(g + 1) * 128], lhsT=kap, rhs=qs[half * 64:(half + 1) * 64, pair, t * 128:(t + 1) * 128],
                            start=True, stop=True), reads=[t_in], writes=[bst])
                for (kind, idx, mi), (bs, bst) in zip(grp, banks_):
                    pb, pt_, _ = PT.next()
                    P.op("act", lambda e, pb=pb, bs=bs: e.activation(out=pb[:], in_=bs[:], func=AF.Exp, scale=0.125),
                         reads=[bst], writes=[pt_])
                    if mi is not None:
                        P.op("pool", lambda e, pb=pb, mi=mi: e.tensor_tensor(
                            out=pb[:].rearrange("p (g c) -> p g c", g=4), in0=pb[:].rearrange("p (g c) -> p g c", g=4),
                            in1=smask[:, mi, :].unsqueeze(1).to_broadcast([128, 4, 128]), op=ALU.mult),
                             reads=[pt_, t_inb], writes=[pt_])
                    pts.append((pb, pt_, kind, idx))
            bo, bot = P.bank()
            for g in range(4):
                for ki, (pb, pt_, kind, idx) in enumerate(pts):
                    vap = vs[:, idx, j, :] if kind == "k" else vh[:, idx, j, :]
                    P.op("pe", lambda e, g=g, vap=vap, pb=pb, bo=bo, ki=ki: e.matmul(
                        out=bo[:, g * 65:(g + 1) * 65], lhsT=pb[:, g * 128:(g + 1) * 128], rhs=vap,
                        start=(ki == 0), stop=(ki == len(pts) - 1)), reads=[pt_, t_in], writes=[bot])
            bo3 = bo[:, 0:260].rearrange("p (g c) -> p g c", g=4)
            P.op("dve", lambda e, bo3=bo3, j=j: e.tensor_tensor(out=den[:, j * 4:(j + 1) * 4], in0=bo3[:, :, 64],
                                                                in1=es_sb[:, j * 4:(j + 1) * 4], op=ALU.add),
                 reads=[bot, t_in], writes=[t_den])
            P.op("dve", lambda e, j=j: e.reciprocal(out=den[:, j * 4:(j + 1) * 4], in_=den[:, j * 4:(j + 1) * 4]),
                 reads=[t_den], writes=[t_den])
            P.op("dve", lambda e, bo3=bo3, j=j: e.tensor_tensor(
                out=mix[:, 512 + j * 256:512 + (j + 1) * 256].rearrange("p (g c) -> p g c", g=4), in0=bo3[:, :, 0:64],
                in1=den[:, j * 4:(j + 1) * 4].unsqueeze(2).to_broadcast([128, 4, 64]), op=ALU.mult),
                 reads=[bot, t_den], writes=[t_mix])

    def gla_tile(t):
        is_ctx = t >= NT_L
        mix, t_mix = get_mix(t)
        ofb, oft_t, ofc = oft.next()
        obb, obt_t, obc = obt.next()
        gb, gt_t, gc = gt_.next()
        P.dma("sp", ofb[:], of_d[t * 128:(t + 1) * 128, :], writes=[oft_t], chan=ofc)
        P.dma("sp", obb[:], ob_d[t * 128:(t + 1) * 128, :], writes=[obt_t], chan=obc)
        P.dma("sp", gb[:], g_d[t * 128:(t + 1) * 128, :], writes=[gt_t], chan=gc)
        P.op("pool", lambda e: e.tensor_tensor(out=og[:], in0=ofb[:], in1=obb[:], op=ALU.add),
             reads=[oft_t, obt_t], writes=[t_og])
        if not is_ctx:
            bf, bft = P.bank()
            for h in range(4):
                hh, pr = h % 2, h // 2
                P.op("pe", lambda e, h=h, hh=hh, pr=pr: e.matmul(
                    out=bf[:, h * 128:(h + 1) * 128], lhsT=qbf[hh * 64:(hh + 1) * 64, pr, t * 128:(t + 1) * 128],
                    rhs=Sinb[0][hh * 64:(hh + 1) * 64, pr, hh * 128:(hh + 1) * 128], start=True, stop=False),
                     reads=[t_in, t_Sin[0]], writes=[bft])
                P.op("pe", lambda e, h=h, hh=hh, pr=pr: e.matmul(
                    out=bf[:, h * 128:(h + 1) * 128], lhsT=qbb[hh * 64:(hh + 1) * 64, pr, t * 128:(t + 1) * 128],
                    rhs=Sinb[1][hh * 64:(hh + 1) * 64, pr, hh * 128:(hh + 1) * 128], start=False, stop=True),
                     reads=[t_in, t_Sin[1]], writes=[bft])
            P.op("dve", lambda e: e.tensor_tensor(out=og[:], in0=og[:], in1=bf[:], op=ALU.add), reads=[t_og, bft], writes=[t_og])
        for h in range(4):
            P.op("act", lambda e, h=h: e.activation(out=junk[:, h * 128:(h + 1) * 128], in_=og[:, h * 128:(h + 1) * 128],
                                                    func=AF.Square, accum_out=ssq4[:, h:h + 1]),
                 reads=[t_og], writes=[t_ssq4, t_junk])
        P.op("act", lambda e: e.activation(out=ssq4[:, 4:8], in_=ssq4[:, 0:4], func=AF.Ln, scale=1.0 / 128, bias=EPS),
             reads=[t_ssq4], writes=[t_ssq4])
        P.op("act", lambda e: e.activation(out=ssq4[:, 4:8], in_=ssq4[:, 4:8], func=AF.Exp, scale=-0.5),
             reads=[t_ssq4], writes=[t_ssq4])
        P.op("act", lambda e: e.activation(out=sgl[:], in_=gb[:], func=AF.Silu), reads=[gt_t], writes=[t_sgl])
        P.op("dve", lambda e: e.tensor_tensor(out=og[:].rearrange("p (h c) -> p h c", h=4),
                                              in0=og[:].rearrange("p (h c) -> p h c", h=4),
                                              in1=ssq4[:, 4:8].unsqueeze(2).to_broadcast([128, 4, 128]), op=ALU.mult),
             reads=[t_og, t_ssq4], writes=[t_og])
        P.op("pool", lambda e: e.tensor_tensor(out=og[:], in0=og[:], in1=gn_sb[:], op=ALU.mult), reads=[t_og, t_in], writes=[t_og])
        P.op("dve", lambda e: e.tensor_tensor(out=mix[:, 0:512], in0=og[:], in1=sgl[:], op=ALU.mult),
             reads=[t_og, t_sgl], writes=[t_mix])

    def post_tile(t):
        is_ctx = t >= NT_L
        mix, t_mix = get_mix(t)
        j = 1 if is_ctx else 0
        xb, xtok, xc = xt.next()
        P.dma("sp", xb[:], x_all[t * 128:(t + 1) * 128, :], writes=[xtok], chan=xc)
        pi = t % 2
        for k in range(8):
            P.cont = k > 0
            P.op("pe", lambda e, k=k: e.transpose(out=ptr[pi][:, k, :], in_=mix[:, k * 128:(k + 1) * 128], identity=id_b[:]),
                 reads=[t_mix, t_idb], writes=[ptr_t[pi]])
        P.op("act", lambda e: e.copy(out=mixT[:], in_=ptr[pi][:]), reads=[ptr_t[pi]], writes=[t_mixT])
        x1b, x1t, x1c = x1.next()
        for cb in range(2):
            bk, bt = P.bank()
            for k in range(8):
                P.cont = k > 0
                P.op("pe", lambda e, k=k, cb=cb, bk=bk: e.matmul(out=bk[:], lhsT=mixT[:, k, :], rhs=wo[:, k, cb * 512:(cb + 1) * 512],
                                                                 start=(k == 0), stop=(k == 7)),
                     reads=[t_mixT, t_inb], writes=[bt])
            P.op("dve", lambda e, cb=cb, bk=bk: e.tensor_tensor(out=x1b[:, cb * 512:(cb + 1) * 512], in0=bk[:],
                                                                in1=MT[:, 0, cb * 512:(cb + 1) * 512], op=ALU.mult),
                 reads=[bt, t_MT], writes=[x1t])
        P.op("pool", lambda e: e.tensor_tensor(out=x1b[:], in0=x1b[:], in1=xb[:], op=ALU.add), reads=[x1t, xtok], writes=[x1t])
        P.dma("sp", x_out[t * 128:(t + 1) * 128, :], x1b[:], reads=[x1t], chan=x1c)
        P.op("act", lambda e: e.activation(out=h2[:], in_=x1b[:], func=AF.Square, accum_out=ssq[:, 0:1]),
             reads=[x1t], writes=[t_ssq, t_h2])
        P.op("act", lambda e: e.activation(out=ssq[:, 1:2], in_=ssq[:, 0:1], func=AF.Ln, scale=1.0 / D, bias=EPS),
             reads=[t_ssq], writes=[t_ssq])
        P.op("act", lambda e: e.activation(out=ssq[:, 1:2], in_=ssq[:, 1:2], func=AF.Exp, scale=-0.5),
             reads=[t_ssq], writes=[t_ssq])
        P.op("dve", lambda e: e.scalar_tensor_tensor(out=h2[:], in0=x1b[:], scalar=ssq[:, 1:2], in1=MT[:, 1, :],
                                                     op0=ALU.mult, op1=ALU.mult), reads=[x1t, t_ssq, t_MT], writes=[t_h2])
        P.op("pool", lambda e: e.tensor_tensor(out=h2[:], in0=h2[:], in1=MT[:, 2, :], op=ALU.add), reads=[t_h2, t_MT], writes=[t_h2])
        for hb in range(2):
            bk, bt = P.bank()
            for k4 in range(4):
                k = hb * 4 + k4
                P.cont = k > 0
                P.op("pe", lambda e, k=k, k4=k4, bk=bk: e.transpose(out=bk[:, k4 * 128:(k4 + 1) * 128], in_=h2[:, k * 128:(k + 1) * 128],
                                                                    identity=id_f[:]), reads=[t_h2, t_c2], writes=[bt])
            P.op("act", lambda e, hb=hb, bk=bk: e.copy(out=h2Tf[:, hb * 4:(hb + 1) * 4, :], in_=bk[:].rearrange("p (a c) -> p a c", a=4)),
                 reads=[bt], writes=[t_h2Tf])
            P.op("dve", lambda e, hb=hb, bk=bk: e.tensor_copy(out=h2T[:, hb * 4:(hb + 1) * 4, t * 128:(t + 1) * 128],
                                                              in_=bk[:].rearrange("p (a c) -> p a c", a=4)),
                 reads=[bt], writes=[t_h2T[t]])
        bl, blt = P.bank()
        for k in range(8):
            P.cont = k > 0
            P.op("pe", lambda e, k=k: e.matmul(out=bl[:, 0:NEXP], lhsT=h2Tf[:, k, :], rhs=wr_sb[:, k, :], start=(k == 0), stop=(k == 7)),
                 reads=[t_h2Tf, t_in], writes=[blt])
        S_, SB, T1, T2, M1, M2 = (rt_[:, i, :] for i in range(6))
        R = rw[:, t, :]
        tk = [t_rt]

        def dv(fn, extra_r=(), extra_w=()):
            P.op("dve", fn, reads=tk + list(extra_r), writes=tk + list(extra_w))
        P.op("act", lambda e: e.activation(out=T1, in_=bl[:, 0:NEXP], func=AF.Exp, scale=-1.0), reads=[blt], writes=tk)
        dv(lambda e: e.tensor_scalar(out=T1, in0=T1, scalar1=1.0, scalar2=None, op0=ALU.add))
        dv(lambda e: e.reciprocal(out=S_, in_=T1))
        dv(lambda e: e.tensor_tensor(out=SB, in0=S_, in1=br_sb[:], op=ALU.add), [t_in])
        sb3 = SB.rearrange("p (g c) -> p g c", g=4)
        t13 = T1.rearrange("p (g c) -> p g c", g=4)
        t23 = T2.rearrange("p (g c) -> p g c", g=4)
        dv(lambda e: e.tensor_reduce(out=M1[:, 0:4], in_=sb3, axis=AX.X, op=ALU.max))
        dv(lambda e: e.tensor_tensor(out=t13, in0=sb3, in1=M1[:, 0:4].unsqueeze(2).to_broadcast([128, 4, 4]), op=ALU.is_equal))
        dv(lambda e: e.scalar_tensor_tensor(out=T2, in0=T1, scalar=-1e9, in1=SB, op0=ALU.mult, op1=ALU.add))
        dv(lambda e: e.tensor_reduce(out=M1[:, 4:8], in_=t23, axis=AX.X, op=ALU.max))
        dv(lambda e: e.tensor_tensor(out=M1[:, 8:12], in0=M1[:, 0:4], in1=M1[:, 4:8], op=ALU.add))
        dv(lambda e: e.tensor_reduce(out=M1[:, 12:13], in_=M1[:, 8:12], axis=AX.X, op=ALU.max))
        dv(lambda e: e.tensor_scalar(out=M2[:, 0:4], in0=M1[:, 8:12], scalar1=M1[:, 12:13], scalar2=None, op0=ALU.is_equal))
        dv(lambda e: e.tensor_scalar(out=M2[:, 0:4], in0=M2[:, 0:4], scalar1=-1.0, scalar2=1e9, op0=ALU.add, op1=ALU.mult))
        dv(lambda e: e.tensor_tensor(out=t13, in0=sb3, in1=M2[:, 0:4].unsqueeze(2).to_broadcast([128, 4, 4]), op=ALU.add))
        dv(lambda e: e.tensor_reduce(out=M2[:, 4:5], in_=T1, axis=AX.X, op=ALU.max))
        dv(lambda e: e.tensor_scalar(out=T2, in0=T1, scalar1=M2[:, 4:5], scalar2=None, op0=ALU.is_equal))
        dv(lambda e: e.scalar_tensor_tensor(out=T1, in0=T2, scalar=-1e9, in1=T1, op0=ALU.mult, op1=ALU.add))
        dv(lambda e: e.tensor_reduce(out=M2[:, 5:6], in_=T1, axis=AX.X, op=ALU.max))
        dv(lambda e: e.tensor_scalar(out=T1, in0=T1, scalar1=M2[:, 5:6], scalar2=None, op0=ALU.is_equal))
        dv(lambda e: e.tensor_tensor(out=T1, in0=T1, in1=T2, op=ALU.add))
        dv(lambda e: e.tensor_tensor(out=T1, in0=T1, in1=S_, op=ALU.mult))
        dv(lambda e: e.tensor_reduce(out=M2[:, 6:7], in_=T1, axis=AX.X, op=ALU.add))
        dv(lambda e: e.reciprocal(out=M2[:, 6:7], in_=M2[:, 6:7]))
        dv(lambda e: e.tensor_scalar(out=R, in0=T1, scalar1=M2[:, 6:7], scalar2=None, op0=ALU.mult), (), [t_rw[t]])

    do_ctx = not (fused and last)
    tiles = ([NT_L, NT_L + 1] if do_ctx else []) + list(range(NT_L))
    if do_ctx:
        build_MT(1)
    swa_tile(tiles[0])
    gla_tile(tiles[0])
    for i, t in enumerate(tiles):
        if i + 1 < len(tiles):
            swa_tile(tiles[i + 1])
            gla_tile(tiles[i + 1])
        if t == 0:
            build_MT(0)
        post_tile(t)
    P.close_scope()

    P.open_scope()
    P.arena_off = persist_end
    acc = P.sb("acc", [128, NT, D], F32)
    t_acc = [P.tok(f"acc{t}") for t in range(NT)]
    wg = Rot(P, "wg", [128, 8, DE], BF16, 2)
    wu = Rot(P, "wu", [128, 8, DE], BF16, 2)
    wd = Rot(P, "wd", [128, 4, D], BF16, 2)
    sg = Rot(P, "sg", [128, 512], F32, 2)
    hid = Rot(P, "hid", [128, 4, 512], BF16, 2)
    hid_toks = [[P.tok(f"hid{i}_{fc}") for fc in range(4)] for i in range(2)]
    groups = [(g * 4, 4) for g in range(4)] + ([(NT_L, 2)] if do_ctx else [])
    xr = Rot(P, "xr", [128, D], F32, 2)
    yo = Rot(P, "yo", [128, D], F32, 2)
    ssq_f = P.sb("ssq_f", [128, 2], F32)
    t_ssqf = P.tok("ssqf")
    junk2 = P.sb("junk2", [128, D], BF16)

    def final_tile(t):
        j = 1 if t >= NT_L else 0
        xb, xtok, xc = xr.next()
        P.dma("sp", xb[:], x_out[t * 128:(t + 1) * 128, :], writes=[xtok], chan=xc)
        P.op("pool", lambda e: e.tensor_tensor(out=acc[:, t, :], in0=acc[:, t, :], in1=G2[:, j, :], op=ALU.mult),
             reads=[t_acc[t], t_c2], writes=[t_acc[t]])
        P.op("dve", lambda e: e.tensor_tensor(out=xb[:], in0=xb[:], in1=acc[:, t, :], op=ALU.add),
             reads=[t_acc[t], xtok], writes=[xtok])
        if t < NT_L and last:
            P.op("act", lambda e: e.activation(out=junk2[:], in_=xb[:], func=AF.Square, accum_out=ssq_f[:, 0:1]),
                 reads=[xtok], writes=[t_ssqf])
            P.op("act", lambda e: e.activation(out=ssq_f[:, 1:2], in_=ssq_f[:, 0:1], func=AF.Ln, scale=1.0 / D, bias=EPS),
                 reads=[t_ssqf], writes=[t_ssqf])
            P.op("act", lambda e: e.activation(out=ssq_f[:, 1:2], in_=ssq_f[:, 1:2], func=AF.Exp, scale=-0.5),
                 reads=[t_ssqf], writes=[t_ssqf])
            yb, yt, yc = yo.next()
            P.op("dve", lambda e: e.scalar_tensor_tensor(out=yb[:], in0=xb[:], scalar=ssq_f[:, 1:2], in1=fn_sb[:],
                                                         op0=ALU.mult, op1=ALU.mult),
                 reads=[xtok, t_ssqf, t_c2], writes=[yt])
            P.dma("sp", y_fin[t * 128:(t + 1) * 128, :], yb[:], reads=[yt], chan=yc)
        P.dma("sp", x_out[t * 128:(t + 1) * 128, :], xb[:], reads=[xtok], chan=xc)

    wbufs = {}
    jstate = {}

    def load_expert(ex):
        wgb, wgt, wgc = wg.next()
        wub, wut, wuc = wu.next()
        wdb, wdt, wdc = wd.next()
        P.dma("pool", wgb[:], w_gate[ex].rearrange("(k p) f -> p k f", p=128), writes=[wgt], chan=wgc)
        P.dma("pool", wub[:], w_up[ex].rearrange("(k p) f -> p k f", p=128), writes=[wut], chan=wuc)
        P.dma("pool", wdb[:], w_down[ex].rearrange("(k p) f -> p k f", p=128), writes=[wdt], chan=wdc)
        wbufs[ex] = (wgb, wgt, wub, wut, wdb, wdt)

    def moe_gu(job, fc):
        ex, t0, ntl = job
        wgb, wgt, wub, wut, wdb, wdt = wbufs[ex]
        n = ntl * 128
        if fc == 0:
            hb0, _, _ = hid.next()
            jstate[job] = (hb0, hid_toks[(hid.i - 1) % 2])
        hb_, htk = jstate[job]
        bg_, bgt = P.bank()
        bu_, but = P.bank()
        for k in range(8):
            P.cont = k > 0
            P.op("pe", lambda e, k=k, fc=fc, bg_=bg_: e.matmul(out=bg_[:, 0:n], lhsT=wgb[:, k, fc * 128:(fc + 1) * 128],
                                                               rhs=h2T[:, k, t0 * 128:t0 * 128 + n], start=(k == 0), stop=(k == 7)),
                 reads=[wgt] + t_h2T[t0:t0 + ntl], writes=[bgt])
            P.cont = k > 0
            P.op("pe", lambda e, k=k, fc=fc, bu_=bu_: e.matmul(out=bu_[:, 0:n], lhsT=wub[:, k, fc * 128:(fc + 1) * 128],
                                                               rhs=h2T[:, k, t0 * 128:t0 * 128 + n], start=(k == 0), stop=(k == 7)),
                 reads=[wut] + t_h2T[t0:t0 + ntl], writes=[but])
        sgb, sgt, _ = sg.next()
        P.op("act", lambda e, sgb=sgb, bg_=bg_: e.activation(out=sgb[:, 0:n], in_=bg_[:, 0:n], func=AF.Silu),
             reads=[bgt], writes=[sgt])
        P.op("dve", lambda e, sgb=sgb, bu_=bu_, fc=fc: e.tensor_tensor(out=hb_[:, fc, 0:n], in0=sgb[:, 0:n], in1=bu_[:, 0:n],
                                                                       op=ALU.mult),
             reads=[sgt, but], writes=[htk[fc]])

    def moe_down(job):
        ex, t0, ntl = job
        wgb, wgt, wub, wut, wdb, wdt = wbufs[ex]
        hb_, htk = jstate[job]
        for tt in range(ntl):
            t = t0 + tt
            bys = [P.bank(), P.bank()]
            for fc in range(4):
                for cb in range(2):
                    by, byt = bys[cb]
                    P.cont = fc > 0
                    P.op("pe", lambda e, fc=fc, cb=cb, by=by, tt=tt: e.matmul(
                        out=by[:], lhsT=hb_[:, fc, tt * 128:(tt + 1) * 128], rhs=wdb[:, fc, cb * 512:(cb + 1) * 512],
                        start=(fc == 0), stop=(fc == 3)), reads=[htk[fc], wdt], writes=[byt])
            for cb in range(2):
                by, byt = bys[cb]
                if ex == 0:
                    P.op("dve", lambda e, cb=cb, by=by, t=t: e.tensor_scalar(
                        out=acc[:, t, cb * 512:(cb + 1) * 512], in0=by[:], scalar1=rw[:, t, 0:1], scalar2=None, op0=ALU.mult),
                         reads=[byt, t_rw[t]], writes=[t_acc[t]])
                else:
                    P.op("dve", lambda e, cb=cb, by=by, t=t: e.scalar_tensor_tensor(
                        out=acc[:, t, cb * 512:(cb + 1) * 512], in0=by[:], scalar=rw[:, t, ex:ex + 1],
                        in1=acc[:, t, cb * 512:(cb + 1) * 512], op0=ALU.mult, op1=ALU.add),
                         reads=[byt, t_rw[t], t_acc[t]], writes=[t_acc[t]])
            if ex == NEXP - 1:
                final_tile(t)

    jobs = [(ex, t0, ntl) for ex in range(NEXP) for (t0, ntl) in groups]
    load_expert(0)
    for fc in range(4):
        moe_gu(jobs[0], fc)
    for i, job in enumerate(jobs):
        nj = jobs[i + 1] if i + 1 < len(jobs) else None
        if nj is not None:
            if nj[0] != job[0]:
                load_expert(nj[0])
            moe_gu(nj, 0)
        moe_down(job)
        if nj is not None:
            for fc in range(1, 4):
                moe_gu(nj, fc)

    if fused:
        P.close_scope()
        return None
    P.emit()
    return nc


IN_OFF = {"lq": (0, 256), "lk": (256, 512), "lv": (512, 1024), "lg": (1024, 1536), "lzf": (1536, 1552),
          "lzb": (1552, 1568), "lsq": (1568, 2080), "lsk": (2080, 2208), "lsv": (2208, 2336)}
_CACHE = {}


def _get(name, fn):
    if name not in _CACHE:
        _CACHE[name] = fn()
    return _CACHE[name]


def _sl(w, key):
    a, b = IN_OFF[key]
    return w[:, a:b]


def host_A_inputs(i, xl, xc, inp):
    w_in = inp["w_in"][i]
    lsk = _sl(w_in, "lsk")
    w_fm = np.ascontiguousarray(np.concatenate(
        [_sl(w_in, "lq"), _sl(w_in, "lk"), _sl(w_in, "lsq"), lsk[:, 0:64], lsk[:, 0:64], lsk[:, 64:128], lsk[:, 64:128]], axis=1))
    w_tm = np.ascontiguousarray(np.concatenate(
        [_sl(w_in, "lv"), _sl(w_in, "lg"), _sl(w_in, "lk"), _sl(w_in, "lzf"), _sl(w_in, "lzb"), _sl(w_in, "lsv")], axis=1))
    up_aug = np.zeros((33, 512), np.float32)
    up_aug[0:16, 0:256] = inp["gla_up_f"][i]
    up_aug[16:32, 256:512] = inp["gla_up_b"][i]
    up_aug[32, 0:256] = inp["gla_bias_f"][i]
    up_aug[32, 256:512] = inp["gla_bias_b"][i]
    cm, mask, perm, ident = _consts_np()
    maps = []
    for r in range(8):
        b, seg = r // 4, r % 4
        cos, sin = _rope_tables(seg * TL, TL)
        maps.append({
            "x_all": np.ascontiguousarray(np.concatenate([xl[b, seg * TL:(seg + 1) * TL], xc[b]], axis=0)),
            "cvec": np.ascontiguousarray(np.stack([inp["c"][b], inp["c_ctx"]], axis=0)),
            "w_ada": inp["w_ada"][i], "b_ada": inp["b_ada"][i], "norm1": inp["norm1"][i],
            "w_fm": w_fm, "w_tm": w_tm, "up_aug": up_aug, "cmats": cm, "masks": mask, "perm": perm, "ident": ident,
            "ropec": cos, "ropes": sin,
        })
    return maps


def run_A(i, xl, xc, inp):
    nc = _get("A", build_A)
    res = run_bass_kernel_spmd(nc, host_A_inputs(i, xl, xc, inp), core_ids=list(range(8)))
    return res.results


def host_B_inputs(i, mapsA, resA, inp):
    cm, mask, perm, ident = _consts_np()
    maps = []
    zk = np.zeros((128, 2, 128), NPBF)
    zv = np.zeros((128, 2, 65), NPBF)
    for r in range(8):
        b, seg = r // 4, r % 4
        ra = resA[r]
        L0 = (NT_L - 1) * 128
        kl = np.asarray(resA[r - 1]["o_ks"])[:, :, L0:L0 + 128] if seg > 0 else zk
        kr = np.asarray(resA[r + 1]["o_ks"])[:, :, 0:128] if seg < 3 else zk
        vl = np.asarray(resA[r - 1]["o_vs"])[L0:L0 + 128] if seg > 0 else zv
        vr = np.asarray(resA[r + 1]["o_vs"])[0:128] if seg < 3 else zv
        flags = np.zeros((128, 8), np.float32)
        for s2 in range(4):
            flags[:, s2] = 1.0 if s2 < seg else 0.0
            flags[:, 4 + s2] = 1.0 if s2 > seg else 0.0
        smask = np.stack([mask[2], mask[3], mask[2] if seg > 0 else 0 * mask[2], mask[3] if seg < 3 else 0 * mask[3]], axis=0)
        maps.append({
            "x_all": mapsA[r]["x_all"], "modr": np.asarray(ra["o_mod"]),
            "qs": np.asarray(ra["o_qs"]), "ks": np.asarray(ra["o_ks"]), "vs": np.asarray(ra["o_vs"]),
            "kh": np.ascontiguousarray(np.stack([kl, kr], axis=2)), "vh": np.ascontiguousarray(np.stack([vl, vr], axis=0)),
            "g": np.asarray(ra["o_g"]), "of": np.asarray(ra["o_of"]), "ob": np.asarray(ra["o_ob"]),
            "qbf": np.asarray(ra["o_qbf"]), "qbb": np.asarray(ra["o_qbb"]),
            "st0": np.ascontiguousarray(np.asarray(ra["o_st"])[0:2]),
            "stF": np.ascontiguousarray(np.stack([np.asarray(resA[b * 4 + s2]["o_st"])[2] for s2 in range(4)], axis=0)),
            "stB": np.ascontiguousarray(np.stack([np.asarray(resA[b * 4 + s2]["o_st"])[3] for s2 in range(4)], axis=0)),
            "pd": np.ascontiguousarray(np.stack([np.asarray(resA[b * 4 + s2]["o_pd"]) for s2 in range(4)], axis=0)),
            "flags": flags, "smask": np.ascontiguousarray(smask.astype(np.float32)), "ident": ident,
            "w_out": inp["w_out"][i], "norm2": inp["norm2"][i], "gnorm": inp["gla_norm"][i], "sink": inp["swa_sink"][i],
            "w_router": inp["w_router"], "b_router": inp["b_router"],
            "w_gate": inp["w_gate"][i], "w_up": inp["w_up"][i], "w_down": inp["w_down"][i], "fnorm": inp["final_norm"],
        })
    return maps


def run_layer(i, xl, xc, inp):
    mapsA = host_A_inputs(i, xl, xc, inp)
    resA = run_bass_kernel_spmd(_get("A", build_A), mapsA, core_ids=list(range(8))).results
    mapsB = host_B_inputs(i, mapsA, resA, inp)
    resB = run_bass_kernel_spmd(_get("B", build_B), mapsB, core_ids=list(range(8))).results
    xo = np.stack([np.asarray(r["x_out"]) for r in resB], axis=0)
    xl2 = xo[:, :TL].reshape(2, 4 * TL, D)
    xc2 = xo[[0, 4], TL:]
    y = np.stack([np.asarray(r["y_fin"]) for r in resB], axis=0).reshape(2, 4 * TL, D)
    return xl2, xc2, y


def build_fused():
    ctx = Ctx()
    for L in range(2):
        build_A(ctx=ctx, L=L)
        build_B(ctx=ctx, L=L, last=(L == 1))
    ctx.P.emit()
    return ctx.nc


def host_fused_inputs(inp):
    cm, mask, perm, ident = _consts_np()
    per_layer = []
    for i in range(2):
        mA = host_A_inputs(i, inp["x"], inp["ctx"], inp)
        per_layer.append(mA)
    maps = []
    for r in range(8):
        b, seg = r // 4, r % 4
        flags = np.zeros((128, 16), np.float32)
        for s2 in range(4):
            flags[:, s2] = 1.0 if s2 < seg else 0.0
            flags[:, 4 + s2] = 1.0 if s2 > seg else 0.0
            flags[:, 8 + s2] = 1.0 if s2 == seg - 1 else 0.0
            flags[:, 12 + s2] = 1.0 if s2 == seg + 1 else 0.0
        smask = np.stack([mask[2], mask[3], mask[2] if seg > 0 else 0 * mask[2], mask[3] if seg < 3 else 0 * mask[3]], axis=0)
        m0 = per_layer[0][r]
        m = {"x_all": m0["x_all"], "cvec": m0["cvec"], "cmats": cm, "masks": mask, "perm": perm, "ident": ident,
             "ropec": m0["ropec"], "ropes": m0["ropes"], "flags16": flags,
             "smask": np.ascontiguousarray(smask.astype(np.float32)),
             "w_router": inp["w_router"], "b_router": inp["b_router"], "fnorm": inp["final_norm"]}
        for i in range(2):
            mi = per_layer[i][r]
            for k in A_LAYER:
                m[f"{k}_{i}"] = mi[k]
            m[f"w_out_{i}"] = inp["w_out"][i]
            m[f"norm2_{i}"] = inp["norm2"][i]
            m[f"gnorm_{i}"] = inp["gla_norm"][i]
            m[f"sink_{i}"] = inp["swa_sink"][i]
            m[f"w_gate_{i}"] = inp["w_gate"][i]
            m[f"w_up_{i}"] = inp["w_up"][i]
            m[f"w_down_{i}"] = inp["w_down"][i]
        maps.append(m)
    return maps


def kernel(**inputs):
    inp = {k: np.ascontiguousarray(np.asarray(v, dtype=np.float32)) for k, v in inputs.items()}
    nc = _get("F", build_fused)
    res = run_bass_kernel_spmd(nc, host_fused_inputs(inp), core_ids=list(range(8))).results
    y = np.stack([np.asarray(r["y_fin"]) for r in res], axis=0).reshape(2, 4 * TL, D)
    return np.ascontiguousarray(y.astype(np.float32))
```
